# Optimizing a Trainium2 kernel written in Bass

```python
import jax, jax.numpy as jnp
from jax import lax
import numpy as np

D_MODEL = 1024
BATCH = 8
SEQ = 2048
DEPTH = 4

N_MIXERS = 3
D_RNN = D_MODEL
LRU_BLOCKS = 8
LRU_BW = D_RNN // LRU_BLOCKS
CONV_WIDTH = 4
LRU_C = 8.0
POOL_WINDOWS = (2, 4, 8, 16)
POOL_GROUPS = len(POOL_WINDOWS)
POOL_GW = D_MODEL // POOL_GROUPS
FOX_HEADS = 16
FOX_HEAD_DIM = D_MODEL // FOX_HEADS
Q_BLOCK = 128
D_FF = 7 * D_MODEL // 2
N_EXPERTS = 8
TOP_K = 2
MOE_ROW_BLOCK = 256
LN_EPS = 1e-5
NEG_INF = -1e30
ALPHA = (2 * DEPTH) ** 0.25
BETA = (8 * DEPTH) ** -0.25
N_A = (DEPTH + 2) // 3
N_B = (DEPTH + 1) // 3
N_C = DEPTH // 3
N_DENSE = (DEPTH + 1) // 2
N_MOE = DEPTH // 2

kernel_name = "hybrid_rglru_pool_fox_moe_deepnorm"


def layer_norm(x, g, b):
    xf = x.astype(jnp.float32)
    mu = jnp.mean(xf, axis=-1, keepdims=True)
    var = jnp.mean(jnp.square(xf - mu), axis=-1, keepdims=True)
    y = (xf - mu) * lax.rsqrt(var + LN_EPS)
    return (y * g.astype(jnp.float32) + b.astype(jnp.float32)).astype(x.dtype)


def causal_dwconv(x, w, b):
    S = x.shape[1]
    xp = jnp.pad(x, ((0, 0), (CONV_WIDTH - 1, 0), (0, 0)))
    y = b
    for k in range(CONV_WIDTH):
        y = y + xp[:, k:k + S] * w[k]
    return y


def _lru_combine(c1, c2):
    a1, b1 = c1
    a2, b2 = c2
    return a1 * a2, a2 * b1 + b2


def rglru_block(x, w_in, conv_w, conv_b, w_gates, b_gates, lam, w_out):
    B, S, _ = x.shape
    u = x @ w_in
    gate_branch, rec_branch = jnp.split(u, 2, axis=-1)
    y_gate = jax.nn.gelu(gate_branch, approximate=True)
    xr = causal_dwconv(rec_branch, conv_w, conv_b)
    xb = xr.reshape(B, S, LRU_BLOCKS, LRU_BW)
    z = jnp.einsum('bsnc,ncg->bsng', xb, w_gates).astype(jnp.float32) + b_gates.astype(jnp.float32)
    r = jax.nn.sigmoid(z[..., :LRU_BW])
    i = jax.nn.sigmoid(z[..., LRU_BW:])
    lam_b = lam.astype(jnp.float32).reshape(LRU_BLOCKS, LRU_BW)
    log_a = -LRU_C * r * jax.nn.softplus(-lam_b)
    a = jnp.exp(log_a)
    mult = jnp.sqrt(-jnp.expm1(2.0 * log_a))
    bterm = mult * (i * xb.astype(jnp.float32))
    a = a.reshape(B, S, D_RNN)
    bterm = bterm.reshape(B, S, D_RNN)
    _, h = lax.associative_scan(_lru_combine, (a, bterm), axis=1)
    return (y_gate * h.astype(x.dtype)) @ w_out


def pool_mixer(x, w, scale):
    B, S, D = x.shape
    xf = x.astype(jnp.float32)
    cs = jnp.cumsum(xf, axis=1)
    t = jnp.arange(1, S + 1, dtype=jnp.float32)[:, None]
    outs = []
    for g, wl in enumerate(POOL_WINDOWS):
        csg = cs[..., g * POOL_GW:(g + 1) * POOL_GW]
        prev = jnp.pad(csg, ((0, 0), (wl, 0), (0, 0)))[:, :S]
        mean = (csg - prev) / jnp.minimum(t, float(wl))
        outs.append(mean - xf[..., g * POOL_GW:(g + 1) * POOL_GW])
    p = jnp.stack(outs, axis=2).astype(x.dtype)
    y = jnp.einsum('bsgc,gcd->bsgd', p, w).reshape(B, S, D)
    return y * scale


def fox_attention(x, w_qkvf, b_f, w_o):
    B, S, D = x.shape
    proj = x @ w_qkvf
    q = proj[..., :D].reshape(B, S, FOX_HEADS, FOX_HEAD_DIM).transpose(0, 2, 1, 3)
    k = proj[..., D:2 * D].reshape(B, S, FOX_HEADS, FOX_HEAD_DIM).transpose(0, 2, 1, 3)
    v = proj[..., 2 * D:3 * D].reshape(B, S, FOX_HEADS, FOX_HEAD_DIM).transpose(0, 2, 1, 3)
    log_f = jax.nn.log_sigmoid(proj[..., 3 * D:].astype(jnp.float32) + b_f.astype(jnp.float32))
    F = jnp.cumsum(log_f, axis=1).transpose(0, 2, 1)
    nq = S // Q_BLOCK
    qb = q.reshape(B, FOX_HEADS, nq, Q_BLOCK, FOX_HEAD_DIM).transpose(2, 0, 1, 3, 4)
    Fq = F.reshape(B, FOX_HEADS, nq, Q_BLOCK).transpose(2, 0, 1, 3)
    qpos = jnp.arange(S, dtype=jnp.int32).reshape(nq, Q_BLOCK)
    kpos = jnp.arange(S, dtype=jnp.int32)
    scale = FOX_HEAD_DIM ** -0.5

    def block(args):
        qi, fi, pi = args
        s = jnp.einsum('bhqd,bhkd->bhqk', qi, k, preferred_element_type=jnp.float32) * scale
        s = s + fi[..., None] - F[:, :, None, :]
        s = jnp.where(kpos[None, :] <= pi[:, None], s, NEG_INF)
        p = jax.nn.softmax(s, axis=-1).astype(v.dtype)
        return jnp.einsum('bhqk,bhkd->bhqd', p, v)

    o = lax.map(block, (qb, Fq, qpos))
    o = o.transpose(1, 0, 3, 2, 4).reshape(B, S, D)
    return o @ w_o


def swiglu(x, w_gu, w_down):
    g, u = jnp.split(x @ w_gu, 2, axis=-1)
    return (jax.nn.silu(g) * u) @ w_down


def moe_swiglu(x, w_router, w_gu, w_down):
    B, S, D = x.shape
    T = B * S
    TK = T * TOP_K
    xt = x.reshape(T, D)
    logits = jnp.einsum('td,de->te', xt, w_router, preferred_element_type=jnp.float32)
    top_val, top_idx = lax.top_k(logits, TOP_K)
    gates = jax.nn.softmax(top_val, axis=-1)
    e_flat = top_idx.reshape(-1).astype(jnp.int32)
    g_flat = gates.reshape(-1)
    tok_flat = jnp.arange(TK, dtype=jnp.int32) // TOP_K
    order = jnp.argsort(e_flat)
    e_sorted = e_flat[order]
    tok_sorted = tok_flat[order]
    g_sorted = g_flat[order]
    counts = jnp.bincount(e_flat, length=N_EXPERTS).astype(jnp.int32)
    padded = (counts + MOE_ROW_BLOCK - 1) // MOE_ROW_BLOCK * MOE_ROW_BLOCK
    start = jnp.cumsum(counts) - counts
    pend = jnp.cumsum(padded)
    pstart = pend - padded
    dest = pstart[e_sorted] + jnp.arange(TK, dtype=jnp.int32) - start[e_sorted]
    n_blocks = (TK + N_EXPERTS * (MOE_ROW_BLOCK - 1) + MOE_ROW_BLOCK - 1) // MOE_ROW_BLOCK
    rows = jnp.zeros((n_blocks * MOE_ROW_BLOCK, D), x.dtype).at[dest].set(xt[tok_sorted])
    block_start = jnp.arange(n_blocks, dtype=jnp.int32) * MOE_ROW_BLOCK
    block_expert = jnp.minimum(jnp.searchsorted(pend, block_start, side='right'), N_EXPERTS - 1)

    def expert_block(args):
        xb, e = args
        return swiglu(xb, w_gu[e], w_down[e])

    y_rows = lax.map(expert_block, (rows.reshape(n_blocks, MOE_ROW_BLOCK, D), block_expert))
    y_sorted = y_rows.reshape(-1, D)[dest] * g_sorted[:, None].astype(x.dtype)
    y = jax.ops.segment_sum(y_sorted, tok_sorted, num_segments=T)
    return y.reshape(B, S, D)


def setup_inputs(seed: int = 0) -> dict:
    key = jax.random.key(seed)
    ks = jax.random.split(key, 24)
    D = D_MODEL
    f32 = jnp.float32

    def nrm(k, shape, s):
        return jax.random.normal(k, shape, f32) * s

    x = nrm(ks[0], (BATCH, SEQ, D), 1.0)
    lru_w_in = nrm(ks[1], (N_A, D, 2 * D_RNN), D ** -0.5)
    lru_conv_w = nrm(ks[2], (N_A, CONV_WIDTH, D_RNN), CONV_WIDTH ** -0.5)
    lru_conv_b = nrm(ks[3], (N_A, D_RNN), 0.02)
    lru_w_gates = nrm(ks[4], (N_A, LRU_BLOCKS, LRU_BW, 2 * LRU_BW), LRU_BW ** -0.5)
    lru_b_gates = nrm(ks[5], (N_A, LRU_BLOCKS, 2 * LRU_BW), 0.1)
    a_c = jax.random.uniform(ks[6], (N_A, D_RNN), f32, 0.9, 0.999)
    a0 = a_c ** (1.0 / LRU_C)
    lru_lambda = jnp.log(a0) - jnp.log1p(-a0)
    lru_w_out = nrm(ks[7], (N_A, D_RNN, D), D_RNN ** -0.5 * BETA)
    pool_w = nrm(ks[8], (N_B, POOL_GROUPS, POOL_GW, POOL_GW), POOL_GW ** -0.5 * BETA)
    pool_scale = 1.0 + nrm(ks[9], (N_B, D), 0.1)
    w_qk = nrm(ks[10], (N_C, D, 2 * D), D ** -0.5)
    w_v = nrm(ks[11], (N_C, D, D), D ** -0.5 * BETA)
    w_f = nrm(ks[12], (N_C, D, FOX_HEADS), D ** -0.5)
    fox_w_qkvf = jnp.concatenate([w_qk, w_v, w_f], axis=-1)
    fox_b_f = jax.random.uniform(ks[13], (N_C, FOX_HEADS), f32, 1.0, 4.0)
    fox_w_o = nrm(ks[14], (N_C, D, D), D ** -0.5 * BETA)
    ffn_w_gu = nrm(ks[15], (N_DENSE, D, 2 * D_FF), D ** -0.5)
    ffn_w_down = nrm(ks[16], (N_DENSE, D_FF, D), D_FF ** -0.5 * BETA)
    moe_router = nrm(ks[17], (N_MOE, D, N_EXPERTS), D ** -0.5)
    moe_w_gu = nrm(ks[18], (N_MOE, N_EXPERTS, D, 2 * D_FF), D ** -0.5)
    moe_w_down = nrm(ks[19], (N_MOE, N_EXPERTS, D_FF, D), D_FF ** -0.5 * BETA)
    ln_mix_g = 1.0 + nrm(ks[20], (DEPTH, D), 0.05)
    ln_mix_b = nrm(ks[21], (DEPTH, D), 0.02)
    ln_ffn_g = 1.0 + nrm(ks[22], (DEPTH, D), 0.05)
    ln_ffn_b = nrm(ks[23], (DEPTH, D), 0.02)
    return {"x": x, "lru_w_in": lru_w_in, "lru_conv_w": lru_conv_w, "lru_conv_b": lru_conv_b,
            "lru_w_gates": lru_w_gates, "lru_b_gates": lru_b_gates, "lru_lambda": lru_lambda,
            "lru_w_out": lru_w_out, "pool_w": pool_w, "pool_scale": pool_scale,
            "fox_w_qkvf": fox_w_qkvf, "fox_b_f": fox_b_f, "fox_w_o": fox_w_o,
            "ffn_w_gu": ffn_w_gu, "ffn_w_down": ffn_w_down, "moe_router": moe_router,
            "moe_w_gu": moe_w_gu, "moe_w_down": moe_w_down, "ln_mix_g": ln_mix_g,
            "ln_mix_b": ln_mix_b, "ln_ffn_g": ln_ffn_g, "ln_ffn_b": ln_ffn_b}


def reference(x, lru_w_in, lru_conv_w, lru_conv_b, lru_w_gates, lru_b_gates, lru_lambda,
              lru_w_out, pool_w, pool_scale, fox_w_qkvf, fox_b_f, fox_w_o, ffn_w_gu, ffn_w_down,
              moe_router, moe_w_gu, moe_w_down, ln_mix_g, ln_mix_b, ln_ffn_g, ln_ffn_b):
    for i in range(DEPTH):
        m = i % N_MIXERS
        j = i // N_MIXERS
        if m == 0:
            h = rglru_block(x, lru_w_in[j], lru_conv_w[j], lru_conv_b[j], lru_w_gates[j],
                            lru_b_gates[j], lru_lambda[j], lru_w_out[j])
        elif m == 1:
            h = pool_mixer(x, pool_w[j], pool_scale[j])
        else:
            h = fox_attention(x, fox_w_qkvf[j], fox_b_f[j], fox_w_o[j])
        x = layer_norm(ALPHA * x + h, ln_mix_g[i], ln_mix_b[i])
        if i % 2 == 0:
            f = swiglu(x, ffn_w_gu[i // 2], ffn_w_down[i // 2])
        else:
            f = moe_swiglu(x, moe_router[i // 2], moe_w_gu[i // 2], moe_w_down[i // 2])
        x = layer_norm(ALPHA * x + f, ln_ffn_g[i], ln_ffn_b[i])
    return x
```

```python
import numpy as np
from contextlib import ExitStack
import concourse.bass as bass
import concourse.mybir as mybir
from concourse.bass_utils import run_bass_kernel_spmd

F32 = mybir.dt.float32
BF16 = mybir.dt.bfloat16
AF = mybir.ActivationFunctionType
ALU = mybir.AluOpType
AX = mybir.AxisListType

D = 1024
S = 2048
NT = 16
NCH = 8
DFF = 3584
NE = 8
ALPHA = float(8 ** 0.25)
LN_EPS = 1e-5
import os
DBG = {k: True for k in os.environ.get("KDBG", "").split(",") if k}
FULL_PLAN = ["lru0", "ffn0", "pool0", "moe0", "fox0", "ffn1", "lru1", "moe1"]

INPUT_SHAPES = {
    "lru_w_in": [2, 1024, 2048], "lru_conv_w": [2, 4, 1024], "lru_conv_b": [2, 1024],
    "lru_w_gates": [2, 8, 128, 256], "lru_b_gates": [2, 8, 256], "lru_lambda": [2, 1024],
    "lru_w_out": [2, 1024, 1024], "pool_w": [1, 4, 256, 256], "pool_scale": [1, 1024],
    "fox_w_qkvf": [1, 1024, 3088], "fox_b_f": [1, 16], "fox_w_o": [1, 1024, 1024],
    "ffn_w_gu": [2, 1024, 7168], "ffn_w_down": [2, 3584, 1024], "moe_router": [2, 1024, 8],
    "moe_w_gu": [2, 8, 1024, 7168], "moe_w_down": [2, 8, 3584, 1024],
    "ln_mix_g": [4, 1024], "ln_mix_b": [4, 1024], "ln_ffn_g": [4, 1024], "ln_ffn_b": [4, 1024],
}


class Tk:
    __slots__ = ("sem", "val", "eng")

    def __init__(self, sem, val, eng):
        self.sem, self.val, self.eng = sem, val, eng


class Buf:
    __slots__ = ("wts", "rts")

    def __init__(self):
        self.wts = {}
        self.rts = {}

    def add_read(self, tk):
        k = id(tk.sem)
        o = self.rts.get(k)
        if o is None or o.val < tk.val:
            self.rts[k] = tk

    def set_write(self, tk):
        k = id(tk.sem)
        o = self.wts.get(k)
        if o is None or o.val < tk.val:
            self.wts[k] = tk
        self.rts = {}


class Prog:
    def __init__(self, nc, es):
        self.nc = nc
        self.es = es
        self.E = {"pe": nc.tensor, "act": nc.scalar, "dve": nc.vector, "pool": nc.gpsimd, "sp": nc.sync}
        self.sem = {k: es.enter_context(nc.semaphore("s_" + k)) for k in self.E}
        self.cnt = {k: 0 for k in self.E}
        self.waited = {k: {} for k in self.E}
        self.dsems = []
        self.dcnt = {}
        self.dpool = {}
        self.dnext = {}

    def wait(self, eng, tk):
        if tk is None or (tk.eng == eng and eng in ("pe", "sp")):
            return
        w = self.waited[eng]
        k = id(tk.sem)
        if w.get(k, -1) >= tk.val:
            return
        self.E[eng].wait_ge(tk.sem, tk.val)
        w[k] = tk.val

    def deps(self, eng, reads, writes):
        for b in reads:
            for t in b.wts.values():
                self.wait(eng, t)
        for b in writes:
            for t in b.wts.values():
                self.wait(eng, t)
            for t in b.rts.values():
                self.wait(eng, t)

    def mark(self, tk, reads, writes):
        for b in reads:
            b.add_read(tk)
        for b in writes:
            b.set_write(tk)

    def op(self, eng, fn, reads=(), writes=()):
        self.deps(eng, reads, writes)
        ins = fn(self.E[eng])
        self.cnt[eng] += 1
        ins.then_inc(self.sem[eng], 1)
        tk = Tk(self.sem[eng], self.cnt[eng], eng)
        self.mark(tk, reads, writes)
        return tk

    def mm(self, out_ap, pairs, reads, writes):
        self.deps("pe", reads, writes)
        n = len(pairs)
        ins = None
        for i, (l, r) in enumerate(pairs):
            ins = self.nc.tensor.matmul(out_ap, lhsT=l, rhs=r, start=(i == 0), stop=(i == n - 1))
        self.cnt["pe"] += 1
        ins.then_inc(self.sem["pe"], 1)
        tk = Tk(self.sem["pe"], self.cnt["pe"], "pe")
        self.mark(tk, reads, writes)
        return tk

    def new_dsem(self, name):
        return None

    def dma(self, q, dsem, out, in_, reads=(), writes=(), **kw):
        pool = self.dpool.setdefault(q, [])
        if len(pool) < 14:
            s = self.es.enter_context(self.nc.semaphore(f"dq_{q}_{len(pool)}"))
            pool.append(s)
            self.dsems.append(s)
            self.dcnt[id(s)] = 0
            self.dnext[q] = 0
        else:
            s = pool[self.dnext[q] % len(pool)]
            self.dnext[q] += 1
        if self.dcnt[id(s)] > 0:
            self.wait(q, Tk(s, self.dcnt[id(s)], None))
        self.deps(q, reads, writes)
        self.E[q].dma_start(out=out, in_=in_, **kw).then_inc(s, 16)
        self.dcnt[id(s)] += 16
        tk = Tk(s, self.dcnt[id(s)], None)
        self.mark(tk, reads, writes)
        return tk

    def barrier(self):
        for e in self.E:
            for o in self.E:
                if o != e and self.cnt[o] > 0:
                    self.wait(e, Tk(self.sem[o], self.cnt[o], o))
            for s in self.dsems:
                if self.dcnt[id(s)] > 0:
                    self.wait(e, Tk(s, self.dcnt[id(s)], None))


def needed_inputs(plan):
    need = {"ln_mix_g", "ln_mix_b", "ln_ffn_g", "ln_ffn_b"}
    for stg in plan:
        if stg.startswith("lru"):
            need |= {k for k in INPUT_SHAPES if k.startswith("lru_")}
            if stg == "lru1":
                need.add("moe_router")
        elif stg == "poolnr":
            need |= {"pool_w", "pool_scale"}
        elif stg == "lnrouter":
            need |= {"moe_router"}
        elif stg.startswith("pool"):
            need |= {"pool_w", "pool_scale", "moe_router"}
        elif stg.startswith("fox"):
            need |= {k for k in INPUT_SHAPES if k.startswith("fox_")}
        elif stg.startswith("ffn"):
            need |= {"ffn_w_gu", "ffn_w_down"}
        elif stg.startswith("moe"):
            need |= {"moe_w_gu", "moe_w_down"}
    return need


def build_program(plan):
    nc = bass.Bass("TRN2", target_bir_lowering=False)
    dram = {"x": nc.dram_tensor("x", [S, D], F32, kind="ExternalInput").ap()}
    for k, shp in INPUT_SHAPES.items():
        if k in needed_inputs(plan):
            dram[k] = nc.dram_tensor(k, shp, F32, kind="ExternalInput").ap()
    out_d = nc.dram_tensor("out", [S, D], F32, kind="ExternalOutput").ap()

    with ExitStack() as es:
        P = Prog(nc, es)

        def sb(name, shape, dt):
            return es.enter_context(nc.sbuf_tensor(name, shape, dt))

        uid = [0]

        def uname(name):
            uid[0] += 1
            return f"{name}_{uid[0]}"

        x_tm = sb("x_tm", [128, NT, D], F32)
        xT = sb("xT", [128, NCH, S], BF16)
        X = [Buf() for _ in range(NT)]
        XT = [Buf() for _ in range(NT)]
        ident = sb("ident", [128, 128], F32)
        Gt = sb("ln_g", [128, D], F32)
        Bt = sb("ln_b", [128, D], F32)
        GB = Buf()
        gate = sb("gate", [128, NT, NE], F32)
        GATE = Buf()
        RTR = {}

        def alloc_router(sbl):
            RTR["eq1"] = sbl("eq1", [128, NT, NE], F32)
            RTR["eq2"] = sbl("eq2", [128, NT, NE], F32)
            RTR["M1"] = sbl("rm1", [128, NT], F32)
            RTR["M2"] = sbl("rm2", [128, NT], F32)
            RTR["RT"] = Buf()
            RTR["wr32"] = sbl("wr32", [128, NCH, NE], F32)
            RTR["WR"] = Buf()
            RTR["xT32"] = [sbl(f"xT32_{i}", [128, NCH, 128], F32) for i in range(2)]
            RTR["XT32"] = [Buf(), Buf()]
            RTR["rsm"] = [sbl(f"rsm{i}", [128, 24], F32) for i in range(2)]
            RTR["RSM"] = [Buf(), Buf()]
        lnst = [sb(f"lnst{i}", [128, 2, 6], F32) for i in range(2)]
        lnmv = [sb(f"lnmv{i}", [128, 8], F32) for i in range(2)]
        LNS = [Buf(), Buf()]
        LNM = [Buf(), Buf()]

        PS = [es.enter_context(nc.psum_tensor(f"ps{i}", [128, 1024], F32)) for i in range(4)]
        PB = [[Buf(), Buf()] for _ in range(4)]

        d_x = P.new_dsem("d_x")
        d_ln = P.new_dsem("d_ln")
        d_misc = P.new_dsem("d_misc")
        d_w = [P.new_dsem(f"d_w{i}") for i in range(2)]
        d_w2 = [P.new_dsem(f"d_v{i}") for i in range(2)]
        d_big = P.new_dsem("d_big")
        d_out = P.new_dsem("d_out")

        IDB = Buf()
        P.op("pool", lambda e: e.memset(ident[:], 1.0), writes=[IDB])
        P.op("pool", lambda e: e.affine_select(out=ident[:], in_=ident[:], pattern=[[1, 128]],
                                               compare_op=ALU.is_equal, fill=0.0, base=0,
                                               channel_multiplier=-1), writes=[IDB])

        xv = dram["x"].rearrange("(i p) d -> p i d", p=128)
        for i4 in range(4):
            P.dma("sp", d_x, x_tm[:, i4 * 4:(i4 + 1) * 4, :], xv[:, i4 * 4:(i4 + 1) * 4, :],
                  writes=X[i4 * 4:(i4 + 1) * 4])

        def emit_xT(i, tp, with_router=False):
            tpb = PB[tp]
            P.deps("pe", [X[i], IDB], tpb)
            ins = None
            for c in range(NCH):
                ins = nc.tensor.transpose(out=PS[tp][:, c * 128:(c + 1) * 128],
                                          in_=x_tm[:, i, c * 128:(c + 1) * 128], identity=ident[:])
            P.cnt["pe"] += 1
            ins.then_inc(P.sem["pe"], 1)
            tk = Tk(P.sem["pe"], P.cnt["pe"], "pe")
            P.mark(tk, [X[i], IDB], tpb)
            src = PS[tp][:, :].rearrange("p (c t) -> p c t", c=NCH)
            P.op("act", lambda e: e.activation(out=xT[:, :, i * 128:(i + 1) * 128], in_=src, func=AF.Copy),
                 reads=tpb, writes=[XT[i]])
            if with_router:
                k = i % 2
                eq1, eq2, M1, M2, RT = RTR["eq1"], RTR["eq2"], RTR["M1"], RTR["M2"], RTR["RT"]
                wr32, WR, xT32, XT32, rsm, RSM = (RTR["wr32"], RTR["WR"], RTR["xT32"], RTR["XT32"],
                                                  RTR["rsm"], RTR["RSM"])
                if DBG.get("noactcopy"):
                    P.op("dve", lambda e: e.memset(xT32[k][:], 0.5), writes=[XT32[k]])
                else:
                    P.op("act", lambda e: e.activation(out=xT32[k][:], in_=src, func=AF.Copy),
                         reads=tpb, writes=[XT32[k]])
                if DBG.get("nologit"):
                    P.op("dve", lambda e: e.tensor_copy(out=PS[1][:, 0:NE], in_=xT32[k][:, 0, 0:NE]),
                         reads=[XT32[k], WR], writes=[PB[1][0]])
                else:
                    P.mm(PS[1][:, 0:NE], [(xT32[k][:, c, :], wr32[:, c, :]) for c in range(NCH)],
                         reads=[XT32[k], WR], writes=[PB[1][0]])
                lg = rsm[k][:, 0:8]
                l2 = rsm[k][:, 8:16]
                if DBG.get("nodve"):
                    return
                P.op("dve", lambda e: e.tensor_copy(out=lg, in_=PS[1][:, 0:NE]),
                     reads=[PB[1][0]], writes=[RSM[k]])
                P.op("dve", lambda e: e.tensor_reduce(out=M1[:, i:i + 1], in_=lg, axis=AX.X, op=ALU.max),
                     reads=[RSM[k]], writes=[RT])
                P.op("dve", lambda e: e.tensor_scalar(out=eq1[:, i, :], in0=lg, scalar1=M1[:, i:i + 1],
                                                      scalar2=None, op0=ALU.is_equal),
                     reads=[RSM[k]], writes=[RT])
                P.op("dve", lambda e: e.scalar_tensor_tensor(out=l2, in0=eq1[:, i, :], scalar=-1e30, in1=lg,
                                                             op0=ALU.mult, op1=ALU.add),
                     reads=[RT], writes=[RSM[k]])
                P.op("dve", lambda e: e.tensor_reduce(out=M2[:, i:i + 1], in_=l2, axis=AX.X, op=ALU.max),
                     reads=[RSM[k]], writes=[RT])
                P.op("dve", lambda e: e.tensor_scalar(out=eq2[:, i, :], in0=l2, scalar1=M2[:, i:i + 1],
                                                      scalar2=None, op0=ALU.is_equal),
                     reads=[RSM[k]], writes=[RT])

        def finish_router():
            if DBG.get("nofinish"):
                return
            eq1, eq2, M1, M2, RT = RTR["eq1"], RTR["eq2"], RTR["M1"], RTR["M2"], RTR["RT"]
            rsm, RSM = RTR["rsm"], RTR["RSM"]
            dm = rsm[0][:, 0:16]
            g1 = rsm[1][:, 0:16]
            P.op("dve", lambda e: e.tensor_tensor(out=dm, in0=M2[:, :], in1=M1[:, :], op=ALU.subtract),
                 reads=[RT], writes=[RSM[0]])
            P.op("act", lambda e: e.activation(out=dm, in_=dm, func=AF.Sigmoid), writes=[RSM[0]])
            P.op("dve", lambda e: e.tensor_scalar(out=g1, in0=dm, scalar1=-1.0, scalar2=1.0,
                                                  op0=ALU.mult, op1=ALU.add),
                 reads=[RSM[0]], writes=[RSM[1]])
            for i in range(NT):
                P.op("dve", lambda e: e.tensor_scalar(out=gate[:, i, :], in0=eq1[:, i, :],
                                                      scalar1=g1[:, i:i + 1], scalar2=None, op0=ALU.mult),
                     reads=[RT, RSM[1]], writes=[GATE])
                P.op("dve", lambda e: e.scalar_tensor_tensor(out=gate[:, i, :], in0=eq2[:, i, :],
                                                             scalar=dm[:, i:i + 1], in1=gate[:, i, :],
                                                             op0=ALU.mult, op1=ALU.add),
                     reads=[RT, RSM[0]], writes=[GATE])

        def load_ln(g_ap, b_ap):
            P.dma("sp", d_ln, Gt[:], g_ap.partition_broadcast(128), writes=[GB])
            P.dma("sp", d_ln, Bt[:], b_ap.partition_broadcast(128), writes=[GB])

        def load_router(w_ap):
            if DBG.get("nordma"):
                P.op("dve", lambda e: e.memset(RTR["wr32"][:], 0.01), writes=[RTR["WR"]])
            else:
                P.dma("sp", d_misc, RTR["wr32"][:], w_ap.rearrange("(c p) e -> p c e", p=128), writes=[RTR["WR"]])

        def ln_pipeline(tiles, hmm, hsrc, tps, with_router=False):
            tps = tps if isinstance(tps, (list, tuple)) else [tps]
            n = len(tiles)

            def p1(t):
                i = tiles[t]
                k = i % 2
                xt = x_tm[:, i, :]
                if hsrc is not None:
                    hmm(i)
                    P.op("dve", lambda e: e.scalar_tensor_tensor(out=xt, in0=xt, scalar=ALPHA, in1=PS[hsrc][:, :],
                                                                 op0=ALU.mult, op1=ALU.add),
                         reads=PB[hsrc], writes=[X[i]])
                P.op("dve", lambda e: e.bn_stats(out=lnst[k][:, 0, :], in_=x_tm[:, i, 0:512]),
                     reads=[X[i]], writes=[LNS[k]])
                P.op("dve", lambda e: e.bn_stats(out=lnst[k][:, 1, :], in_=x_tm[:, i, 512:1024]),
                     reads=[X[i]], writes=[LNS[k]])
                mv = lnmv[k]
                P.op("dve", lambda e: e.bn_aggr(out=mv[:, 0:2], in_=lnst[k][:].rearrange("p a b -> p (a b)")),
                     reads=[LNS[k]], writes=[LNM[k]])
                P.op("dve", lambda e: e.tensor_scalar(out=mv[:, 2:3], in0=mv[:, 1:2], scalar1=LN_EPS, scalar2=None,
                                                      op0=ALU.add), reads=[LNM[k]], writes=[LNM[k]])
                P.op("act", lambda e: e.activation(out=mv[:, 3:4], in_=mv[:, 2:3], func=AF.Sqrt),
                     reads=[LNM[k]], writes=[LNM[k]])

            def p2(t):
                i = tiles[t]
                k = i % 2
                xt = x_tm[:, i, :]
                mv = lnmv[k]
                P.op("dve", lambda e: e.reciprocal(out=mv[:, 4:5], in_=mv[:, 3:4]), reads=[LNM[k]], writes=[LNM[k]])
                P.op("dve", lambda e: e.tensor_scalar(out=mv[:, 5:6], in0=mv[:, 0:1], scalar1=mv[:, 4:5], scalar2=-1.0,
                                                      op0=ALU.mult, op1=ALU.mult), reads=[LNM[k]], writes=[LNM[k]])
                P.op("act", lambda e: e.activation(out=xt, in_=xt, func=AF.Identity, scale=mv[:, 4:5], bias=mv[:, 5:6]),
                     reads=[LNM[k]], writes=[X[i]])

            def p3(t):
                i = tiles[t]
                xt = x_tm[:, i, :]
                P.op("dve", lambda e: e.tensor_tensor(out=xt, in0=xt, in1=Gt[:], op=ALU.mult),
                     reads=[GB], writes=[X[i]])
                P.op("pool", lambda e: e.tensor_tensor(out=xt, in0=xt, in1=Bt[:], op=ALU.add),
                     reads=[GB], writes=[X[i]])

            def p4(t):
                i = tiles[t]
                emit_xT(i, tps[t % len(tps)], with_router)

            for t in range(n + 3):
                if t < n:
                    p1(t)
                if 0 <= t - 1 < n:
                    p2(t - 1)
                if 0 <= t - 2 < n:
                    p3(t - 2)
                if 0 <= t - 3 < n:
                    p4(t - 3)

        for i in range(NT):
            emit_xT(i, 2 + (i % 2))

        def ffn_stage(wgu_aps, wd_aps, use_gate, g_ap, b_ap):
            with ExitStack() as st:
                def sbl(name, shape, dt):
                    return st.enter_context(nc.sbuf_tensor(uname(name), shape, dt))
                WG = [sbl(f"f_wg{i}", [128, NCH, 512], BF16) for i in range(2)]
                WU = [sbl(f"f_wu{i}", [128, NCH, 512], BF16) for i in range(2)]
                WD = [sbl(f"f_wd{i}", [128, 4, D], BF16) for i in range(2)]
                WGB = [[Buf() for _ in range(4)] for _ in range(2)]
                WDB = [Buf(), Buf()]
                hT = [sbl(f"f_hT{i}", [128, 4, 512], BF16) for i in range(2)]
                HB = [Buf(), Buf()]
                sg = [sbl(f"f_sg{i}", [128, 512], F32) for i in range(2)]
                SG = [Buf(), Buf()]

                load_ln(g_ap, b_ap)
                for i in range(NT):
                    P.op("act", lambda e: e.activation(out=x_tm[:, i, :], in_=x_tm[:, i, :], func=AF.Copy, scale=ALPHA),
                         writes=[X[i]])
                groups = [(e_, G) for e_ in range(len(wgu_aps)) for G in range(7)]

                def load_w(gi):
                    e_, G = groups[gi]
                    k = gi % 2
                    wgu = wgu_aps[e_].rearrange("(c p) n -> p c n", p=128)
                    wdn = wd_aps[e_].rearrange("(j p) n -> p j n", p=128)
                    if gi == 0:
                        for jj in range(4):
                            c0 = G * 512 + jj * 128
                            P.dma("pool", d_w[k], WG[k][:, :, jj * 128:(jj + 1) * 128], wgu[:, :, c0:c0 + 128],
                                  writes=[WGB[k][jj]])
                            P.dma("pool", d_w[k], WU[k][:, :, jj * 128:(jj + 1) * 128],
                                  wgu[:, :, DFF + c0:DFF + c0 + 128], writes=[WGB[k][jj]])
                    else:
                        P.dma("pool", d_w[k], WG[k][:], wgu[:, :, G * 512:(G + 1) * 512], writes=WGB[k])
                        P.dma("pool", d_w[k], WU[k][:], wgu[:, :, DFF + G * 512:DFF + (G + 1) * 512], writes=WGB[k])
                    P.dma("pool", d_w[k], WD[k][:], wdn[:, G * 4:(G + 1) * 4, :], writes=[WDB[k]])

                steps = [(gi, tb) for gi in range(len(groups)) for tb in range(4)]
                cnt = [0]

                def gu(s):
                    gi, tb = steps[s]
                    k = gi % 2
                    hk = s % 2
                    xin = [XT[tb * 4 + t] for t in range(4)]
                    for jj in range(4):
                        q = cnt[0] % 2
                        cnt[0] += 1
                        P.mm(PS[q][:, 0:512],
                             [(WG[k][:, c, jj * 128:(jj + 1) * 128], xT[:, c, tb * 512:(tb + 1) * 512])
                              for c in range(NCH)], reads=[WGB[k][jj]] + xin, writes=[PB[q][0]])
                        P.mm(PS[q][:, 512:1024],
                             [(WU[k][:, c, jj * 128:(jj + 1) * 128], xT[:, c, tb * 512:(tb + 1) * 512])
                              for c in range(NCH)], reads=[WGB[k][jj]] + xin, writes=[PB[q][1]])
                        P.op("act", lambda e: e.activation(out=sg[q][:], in_=PS[q][:, 0:512], func=AF.Silu),
                             reads=[PB[q][0]], writes=[SG[q]])
                        P.op("dve", lambda e: e.tensor_tensor(out=hT[hk][:, jj, :], in0=sg[q][:],
                                                              in1=PS[q][:, 512:1024], op=ALU.mult),
                             reads=[SG[q], PB[q][1]], writes=[HB[hk]])

                def down(s):
                    gi, tb = steps[s]
                    e_, G = groups[gi]
                    k = gi % 2
                    hk = s % 2
                    for tt in range(4):
                        i = tb * 4 + tt
                        y = 2 + (tt % 2)
                        for half in range(2):
                            P.mm(PS[y][:, half * 512:(half + 1) * 512],
                                 [(hT[hk][:, jj, tt * 128:(tt + 1) * 128], WD[k][:, jj, half * 512:(half + 1) * 512])
                                  for jj in range(4)], reads=[WDB[k], HB[hk]], writes=[PB[y][half]])
                        sc = gate[:, i, e_:e_ + 1] if use_gate else 1.0
                        rd = PB[y] + ([GATE] if use_gate else [])
                        P.op("dve", lambda e: e.scalar_tensor_tensor(out=x_tm[:, i, :], in0=PS[y][:, :], scalar=sc,
                                                                     in1=x_tm[:, i, :], op0=ALU.mult, op1=ALU.add),
                             reads=rd, writes=[X[i]])

                def tail_ln(s_done):
                    gi_d, tb_d = steps[s_done]
                    if gi_d == len(groups) - 1:
                        ln_pipeline([tb_d * 4 + t for t in range(4)], None, None, [2, 3], False)

                load_w(0)
                for s in range(len(steps)):
                    gi, tb = steps[s]
                    gu(s)
                    if s > 0:
                        down(s - 1)
                        tail_ln(s - 1)
                    if tb == 0 and gi + 1 < len(groups):
                        load_w(gi + 1)
                down(len(steps) - 1)
                tail_ln(len(steps) - 1)
                return

        def ffn_ln():
            ln_pipeline(list(range(NT)), None, None, [2, 3], False)

        def lru_stage(j, li, router_w):
            with ExitStack() as st:
                def sbl(name, shape, dt):
                    return st.enter_context(nc.sbuf_tensor(uname(name), shape, dt))
                praw = sbl("l_praw", [64, 128], F32)
                prm = sbl("l_prm", [128, 64], F32)
                prh = sbl("l_prh", [128, 16], F32)
                nsp = sbl("l_nsp", [128, 8], F32)
                nsph = sbl("l_nsph", [128, 8], F32)
                PRM = Buf()
                wout = sbl("l_wout", [128, NCH, D], BF16)
                WOUT = Buf()
                wgt = sbl("l_wg", [128, 8, 256], BF16)
                WGT = Buf()
                win = sbl("l_win", [128, NCH, 2 * D], BF16)
                WINQ = [Buf() for _ in range(4)]
                mT = [sbl("l_mT0", [128, NCH, 512], BF16)]
                MT = [Buf()]
                halo = sbl("l_halo", [128, 8, 4], F32)
                hlast = sbl("l_hlast", [128, 8], F32)
                HALO = [Buf() for _ in range(8)]
                HL = [Buf() for _ in range(8)]

                def tmp2(name, shape, dt):
                    return [sbl(f"{name}{i}", shape, dt) for i in range(2)], [Buf(), Buf()]
                yg = [sbl(f"l_yg{i}", [128, 512], BF16) for i in range(3)]
                YG = [Buf(), Buf(), Buf()]
                rec, REC = tmp2("l_rec", [128, 516], F32)
                xr, XR = tmp2("l_xr", [128, 512], F32)
                xrb, XRB = tmp2("l_xrb", [128, 512], BF16)
                ra, RA = tmp2("l_ra", [128, 512], F32)
                it, IT = tmp2("l_it", [128, 512], F32)
                mu, MU = tmp2("l_mu", [128, 512], F32)
                hh, HH = tmp2("l_hh", [128, 512], F32)

                load_ln(dram["ln_mix_g"][li], dram["ln_mix_b"][li])
                if router_w is not None:
                    alloc_router(sbl)
                    load_router(router_w)
                PRAW = Buf()
                P.dma("sp", d_misc, praw[0:32, :], dram["lru_conv_w"][j].rearrange("k (c p) -> (k c) p", p=128),
                      writes=[PRAW])
                P.dma("sp", d_misc, praw[32:40, :], dram["lru_conv_b"][j].rearrange("(c p) -> c p", p=128),
                      writes=[PRAW])
                P.dma("sp", d_misc, praw[40:48, :], dram["lru_lambda"][j].rearrange("(c p) -> c p", p=128),
                      writes=[PRAW])
                P.dma("sp", d_misc, praw[48:64, :], dram["lru_b_gates"][j].rearrange("n (h p) -> (n h) p", p=128),
                      writes=[PRAW])
                winv = dram["lru_w_in"][j].rearrange("(c p) n -> p c n", p=128)
                for q4 in (0, 2, 1, 3):
                    P.dma("pool", d_big, win[:, :, q4 * 512:(q4 + 1) * 512], winv[:, :, q4 * 512:(q4 + 1) * 512],
                          writes=[WINQ[q4]])
                P.dma("pool", d_big, wgt[:], dram["lru_w_gates"][j].rearrange("n p g -> p n g"), writes=[WGT])
                P.dma("pool", d_big, wout[:], dram["lru_w_out"][j].rearrange("(c p) n -> p c n", p=128),
                      writes=[WOUT])
                P.deps("pe", [PRAW, IDB], PB[3])
                ins = nc.tensor.transpose(out=PS[3][:, 0:64], in_=praw[:, :], identity=ident[0:64, 0:64])
                P.cnt["pe"] += 1
                ins.then_inc(P.sem["pe"], 1)
                tk = Tk(P.sem["pe"], P.cnt["pe"], "pe")
                P.mark(tk, [PRAW, IDB], PB[3])
                P.op("dve", lambda e: e.tensor_copy(out=prm[:], in_=PS[3][:, 0:64]), reads=PB[3], writes=[PRM])
                P.op("dve", lambda e: e.tensor_scalar(out=prh[:], in0=prm[:, 48:64], scalar1=0.5, scalar2=None,
                                                      op0=ALU.mult), writes=[PRM])
                P.op("act", lambda e: e.activation(out=nsp[:], in_=prm[:, 40:48], func=AF.Exp, scale=-1.0),
                     reads=[PRM], writes=[PRM])
                P.op("act", lambda e: e.activation(out=nsp[:], in_=nsp[:], func=AF.Ln, bias=1.0),
                     reads=[PRM], writes=[PRM])
                P.op("dve", lambda e: e.tensor_scalar(out=nsp[:], in0=nsp[:], scalar1=-8.0, scalar2=None,
                                                      op0=ALU.mult), reads=[PRM], writes=[PRM])
                P.op("dve", lambda e: e.tensor_scalar(out=nsph[:], in0=nsp[:], scalar1=0.5, scalar2=None,
                                                      op0=ALU.mult), writes=[PRM])
                P.op("dve", lambda e: e.memset(halo[:], 0.0), writes=HALO)
                P.op("dve", lambda e: e.memset(hlast[:], 0.0), writes=HL)

                steps = [(tb, c) for tb in range(4) for c in range(8)]

                def mm_gr(s):
                    tb, c = steps[s]
                    q = s % 2
                    xin = [XT[tb * 4 + t] for t in range(4)]
                    t0 = tb * 512
                    P.mm(PS[q][:, 0:512], [(win[:, cc, c * 128:(c + 1) * 128], xT[:, cc, t0:t0 + 512])
                                           for cc in range(NCH)], reads=[WINQ[c // 4]] + xin, writes=[PB[q][0]])
                    P.mm(PS[q][:, 512:1024], [(win[:, cc, D + c * 128:D + (c + 1) * 128], xT[:, cc, t0:t0 + 512])
                                              for cc in range(NCH)], reads=[WINQ[2 + c // 4]] + xin, writes=[PB[q][1]])

                def part_a(s):
                    tb, c = steps[s]
                    k = s % 2
                    q = s % 2
                    P.op("act", lambda e: e.activation(out=yg[s % 3][:], in_=PS[q][:, 0:512], func=AF.Gelu_apprx_tanh),
                         reads=[PB[q][0]], writes=[YG[s % 3]])
                    P.op("pool", lambda e: e.tensor_copy(out=rec[k][:, 0:4], in_=halo[:, c, :]),
                         reads=[HALO[c]], writes=[REC[k]])
                    P.op("dve", lambda e: e.tensor_copy(out=rec[k][:, 4:516], in_=PS[q][:, 512:1024]),
                         reads=[PB[q][1]], writes=[REC[k]])

                def c_dve(s):
                    tb, c = steps[s]
                    k = s % 2
                    q = s % 2
                    P.op("pool", lambda e: e.tensor_copy(out=halo[:, c, :], in_=rec[k][:, 512:516]),
                         reads=[REC[k]], writes=[HALO[c]])
                    P.op("dve", lambda e: e.tensor_scalar(out=xr[k][:], in0=rec[k][:, 4:516],
                                                          scalar1=prm[:, 24 + c:25 + c], scalar2=prm[:, 32 + c:33 + c],
                                                          op0=ALU.mult, op1=ALU.add),
                         reads=[REC[k], PRM], writes=[XR[k]])
                    for kk in range(3):
                        sh = 3 - kk
                        P.op("dve", lambda e: e.scalar_tensor_tensor(out=xr[k][:], in0=rec[k][:, 4 - sh:516 - sh],
                                                                     scalar=prm[:, kk * 8 + c:kk * 8 + c + 1],
                                                                     in1=xr[k][:], op0=ALU.mult, op1=ALU.add),
                             reads=[REC[k], PRM], writes=[XR[k]])

                def c_act(s):
                    tb, c = steps[s]
                    k = s % 2
                    q = s % 2
                    P.op("act", lambda e: e.activation(out=xrb[k][:], in_=xr[k][:], func=AF.Copy),
                         reads=[XR[k]], writes=[XRB[k]])
                    P.mm(PS[q][:, 0:512], [(wgt[:, c, 0:128], xrb[k][:])], reads=[WGT, XRB[k]], writes=[PB[q][0]])
                    P.mm(PS[q][:, 512:1024], [(wgt[:, c, 128:256], xrb[k][:])], reads=[WGT, XRB[k]],
                         writes=[PB[q][1]])

                def e_act(s):
                    tb, c = steps[s]
                    k = s % 2
                    q = s % 2
                    P.op("act", lambda e: e.activation(out=ra[k][:], in_=PS[q][:, 0:512], func=AF.Tanh, scale=0.5,
                                                       bias=prh[:, 2 * c:2 * c + 1]),
                         reads=[PB[q][0], PRM], writes=[RA[k]])
                    P.op("act", lambda e: e.activation(out=it[k][:], in_=PS[q][:, 512:1024], func=AF.Tanh, scale=0.5,
                                                       bias=prh[:, 2 * c + 1:2 * c + 2]),
                         reads=[PB[q][1], PRM], writes=[IT[k]])
                    P.op("act", lambda e: e.activation(out=mu[k][:], in_=ra[k][:], func=AF.Exp, scale=nsp[:, c:c + 1],
                                                       bias=nsp[:, c:c + 1]),
                         reads=[RA[k], PRM], writes=[MU[k]])
                    P.op("act", lambda e: e.activation(out=ra[k][:], in_=ra[k][:], func=AF.Exp, scale=nsph[:, c:c + 1],
                                                       bias=nsph[:, c:c + 1]),
                         reads=[PRM], writes=[RA[k]])
                    P.op("act", lambda e: e.activation(out=mu[k][:], in_=mu[k][:], func=AF.Ln, scale=-1.0, bias=1.0),
                         writes=[MU[k]])
                    P.op("act", lambda e: e.activation(out=mu[k][:], in_=mu[k][:], func=AF.Exp, scale=0.5),
                         writes=[MU[k]])

                def f_dve(s):
                    tb, c = steps[s]
                    k = s % 2
                    q = s % 2
                    P.op("dve", lambda e: e.scalar_tensor_tensor(out=it[k][:], in0=it[k][:], scalar=1.0, in1=xr[k][:],
                                                                 op0=ALU.add, op1=ALU.mult),
                         reads=[XR[k]], writes=[IT[k]])
                    P.op("dve", lambda e: e.scalar_tensor_tensor(out=mu[k][:], in0=mu[k][:], scalar=0.5, in1=it[k][:],
                                                                 op0=ALU.mult, op1=ALU.mult),
                         reads=[IT[k]], writes=[MU[k]])
                    P.op("dve", lambda e: e.tensor_tensor_scan(out=hh[k][:], data0=ra[k][:], data1=mu[k][:],
                                                               initial=hlast[:, c:c + 1], op0=ALU.mult, op1=ALU.add),
                         reads=[RA[k], MU[k], HL[c]], writes=[HH[k]])
                    P.op("dve", lambda e: e.tensor_copy(out=hlast[:, c:c + 1], in_=hh[k][:, 511:512]),
                         reads=[HH[k]], writes=[HL[c]])
                    P.op("pool", lambda e: e.tensor_tensor(out=mT[0][:, c, :], in0=yg[s % 3][:], in1=hh[k][:], op=ALU.mult),
                         reads=[YG[s % 3], HH[k]], writes=[MT[0]])

                mm_gr(0)
                part_a(0)
                c_dve(0)
                c_act(0)
                for s, (tb, c) in enumerate(steps):
                    if s + 1 < len(steps):
                        mm_gr(s + 1)
                        part_a(s + 1)
                        c_dve(s + 1)
                    e_act(s)
                    if s + 1 < len(steps):
                        c_act(s + 1)
                    f_dve(s)
                    if c == 7:
                        tiles = [tb * 4 + tt for tt in range(4)]

                        def hmm(i):
                            tt = i % 4
                            for half in range(2):
                                P.mm(PS[2][:, half * 512:(half + 1) * 512],
                                     [(mT[0][:, cc, tt * 128:(tt + 1) * 128], wout[:, cc, half * 512:(half + 1) * 512])
                                      for cc in range(NCH)], reads=[MT[0], WOUT], writes=[PB[2][half]])
                        ln_pipeline(tiles, hmm, 2, 3, router_w is not None)
                if router_w is not None:
                    finish_router()

        def pool_stage(li, router_w):
            with ExitStack() as st:
                def sbl(name, shape, dt):
                    return st.enter_context(nc.sbuf_tensor(uname(name), shape, dt))
                pT = sbl("p_pT", [128, NCH, S], BF16)
                PT_ = [Buf() for _ in range(NCH)]
                pw32 = sbl("p_w32", [128, 4, 2, 256], F32)
                pwb = sbl("p_wb", [128, 4, 2, 256], BF16)
                scb = sbl("p_scb", [128, D], F32)
                PW = Buf()
                rc16 = sbl("p_rc", [128, 16], F32)
                RC = Buf()
                A = [sbl(f"p_A{i}", [128, S], F32) for i in range(2)]
                Bq = [sbl(f"p_B{i}", [128, S], F32) for i in range(2)]
                AB = [Buf(), Buf()]
                tsm = [sbl(f"p_t{i}", [128, 16], F32) for i in range(2)]

                load_ln(dram["ln_mix_g"][li], dram["ln_mix_b"][li])
                if router_w is not None:
                    alloc_router(sbl)
                    load_router(router_w)
                P.dma("sp", d_misc, pw32[:], dram["pool_w"][0].rearrange("g (k p) d -> p g k d", p=128), writes=[PW])
                P.dma("sp", d_misc, scb[:], dram["pool_scale"][0].partition_broadcast(128), writes=[PW])
                for g in range(4):
                    for kc in range(2):
                        P.op("dve", lambda e: e.tensor_tensor(out=pwb[:, g, kc, :], in0=pw32[:, g, kc, :],
                                                              in1=scb[:, g * 256:(g + 1) * 256], op=ALU.mult),
                             reads=[PW], writes=[PW])
                for t in range(16):
                    P.op("dve", lambda e: e.memset(rc16[:, t:t + 1], 1.0 / (t + 1)), writes=[RC])
                for c in range(NCH):
                    g = c // 2
                    wl = 2 ** (g + 1)
                    k = c % 2
                    eng = "dve"
                    xc = xT[:, c, :]
                    srcb = xc
                    bufs2 = [A[k], Bq[k]]
                    bi = 0
                    sh = 1
                    while sh < wl:
                        dst = bufs2[bi]
                        P.op(eng, lambda e: e.tensor_tensor(out=dst[:, sh:S], in0=srcb[:, sh:S], in1=srcb[:, 0:S - sh],
                                                            op=ALU.add), reads=XT, writes=[AB[k]])
                        P.op(eng, lambda e: e.tensor_copy(out=dst[:, 0:sh], in_=srcb[:, 0:sh]), reads=XT, writes=[AB[k]])
                        srcb = dst
                        bi ^= 1
                        sh *= 2
                    cur = srcb
                    P.op("dve", lambda e: e.scalar_tensor_tensor(out=pT[:, c, :], in0=cur[:, :], scalar=1.0 / wl,
                                                                 in1=xc, op0=ALU.mult, op1=ALU.subtract),
                         reads=[AB[k]] + XT, writes=[PT_[c]])
                    if wl > 2:
                        n = wl - 1
                        P.op("dve", lambda e: e.tensor_tensor(out=tsm[k][:, 0:n], in0=cur[:, 0:n], in1=rc16[:, 0:n],
                                                              op=ALU.mult), reads=[AB[k], RC], writes=[AB[k]])
                        P.op("dve", lambda e: e.tensor_tensor(out=pT[:, c, 0:n], in0=tsm[k][:, 0:n], in1=xT[:, c, 0:n],
                                                              op=ALU.subtract), reads=[AB[k]], writes=[PT_[c]])
                    else:
                        P.op("dve", lambda e: e.memset(pT[:, c, 0:1], 0.0), writes=[PT_[c]])
                def hmm(i):
                    for g in range(4):
                        half = g // 2
                        P.mm(PS[2][:, g * 256:(g + 1) * 256],
                             [(pT[:, 2 * g + kc, i * 128:(i + 1) * 128], pwb[:, g, kc, :]) for kc in range(2)],
                             reads=[PT_[2 * g], PT_[2 * g + 1], PW], writes=[PB[2][half]])
                ln_pipeline(list(range(NT)), hmm, 2, [3, 0], router_w is not None)
                if router_w is not None:
                    finish_router()

        def fox_stage(li):
            wqkvf = dram["fox_w_qkvf"][0].rearrange("(c p) n -> p c n", p=128)
            with ExitStack() as st0:
                oT = st0.enter_context(nc.sbuf_tensor(uname("a_oT"), [128, NCH, S], BF16))
                OT = Buf()
                load_ln(dram["ln_mix_g"][li], dram["ln_mix_b"][li])
                with ExitStack() as st:
                    def sbl(name, shape, dt):
                        return st.enter_context(nc.sbuf_tensor(uname(name), shape, dt))
                    wf = sbl("a_wf", [128, NCH, 16], BF16)
                    WF = Buf()
                    nbf = sbl("a_nbf", [16, 1], F32)
                    NBF = Buf()
                    ones16 = sbl("a_ones", [16, 512], F32)
                    spT = sbl("a_spT", [16, 512], F32)
                    SPT = Buf()
                    Fneg = sbl("a_Fneg", [16, S], F32)
                    FN = Buf()
                    Fk = sbl("a_Fk", [128, NT, 16], F32)
                    FK = Buf()
                    neg1 = sbl("a_neg1", [16, 128], F32)
                    sel = [sbl(f"a_sel{i}", [16, 128], F32) for i in range(2)]
                    SEL = [Buf(), Buf()]
                    NEG1 = Buf()
                    wq2 = [sbl(f"a_wq{i}", [128, NCH, 128], BF16) for i in range(2)]
                    wk2 = [sbl(f"a_wk{i}", [128, NCH, 128], BF16) for i in range(2)]
                    wv2 = [sbl(f"a_wv{i}", [128, NCH, 128], BF16) for i in range(2)]
                    WQ2 = [Buf(), Buf()]

                    def load_qkv(cq):
                        kq = cq % 2
                        P.dma("pool", d_w[0], wq2[kq][:], wqkvf[:, :, cq * 128:(cq + 1) * 128], writes=[WQ2[kq]])
                        P.dma("pool", d_w[0], wk2[kq][:], wqkvf[:, :, D + cq * 128:D + (cq + 1) * 128],
                              writes=[WQ2[kq]])
                        P.dma("pool", d_w[0], wv2[kq][:], wqkvf[:, :, 2 * D + cq * 128:2 * D + (cq + 1) * 128],
                              writes=[WQ2[kq]])
                    qT = sbl("a_qT", [128, S], BF16)
                    kT = sbl("a_kT", [128, S], BF16)
                    QK = Buf()
                    vc = sbl("a_vc", [128, NT, 2, 65], BF16)
                    VC = Buf()
                    Fq = [sbl(f"a_Fq{i}", [128, 512], F32) for i in range(2)]
                    FQ = [Buf(), Buf()]
                    tmp = [sbl(f"a_tmp{i}", [128, 512], F32) for i in range(4)]
                    TMP = [Buf() for _ in range(4)]
                    pt = [sbl(f"a_pt{i}", [128, 512], BF16) for i in range(4)]
                    PTB = [Buf() for _ in range(4)]
                    SPS = [(PS[0], 0, PB[0][0]), (PS[0], 512, PB[0][1]), (PS[3], 512, PB[3][1]), (PS[3], 0, PB[3][0])]
                    NS = len(SPS)
                    otm = sbl("a_otm", [128, NT, 128], F32)
                    OTM = Buf()
                    rs = sbl("a_rs", [128, 8], F32)
                    RS = Buf()

                    P.dma("pool", d_big, wf[:], wqkvf[:, :, 3 * D:3 * D + 16], writes=[WF])
                    P.dma("sp", d_misc, nbf[:], dram["fox_b_f"][0].rearrange("(h o) -> h o", o=1), writes=[NBF])
                    P.op("dve", lambda e: e.tensor_scalar(out=nbf[:], in0=nbf[:], scalar1=-1.0, scalar2=None,
                                                          op0=ALU.mult), writes=[NBF])
                    P.op("pool", lambda e: e.memset(ones16[:], 1.0), writes=[SPT])
                    P.op("pool", lambda e: e.memset(neg1[:], -1.0), writes=[NEG1])
                    P.op("pool", lambda e: e.memset(vc[:], 1.0), writes=[VC])
                    for tb in range(4):
                        xin = [XT[tb * 4 + t] for t in range(4)]
                        P.mm(PS[3][0:16, 0:512], [(wf[:, c, :], xT[:, c, tb * 512:(tb + 1) * 512]) for c in range(NCH)],
                             reads=[WF] + xin, writes=[PB[3][0]])
                        P.op("act", lambda e: e.activation(out=spT[:], in_=PS[3][0:16, 0:512],
                                                           func=AF.Exp, scale=-1.0, bias=nbf[:, 0:1]),
                             reads=[PB[3][0], NBF], writes=[SPT])
                        P.op("act", lambda e: e.activation(out=spT[:], in_=spT[:], func=AF.Ln, bias=1.0), writes=[SPT])
                        init = 0.0 if tb == 0 else Fneg[:, tb * 512 - 1:tb * 512]
                        P.op("dve", lambda e: e.tensor_tensor_scan(out=Fneg[:, tb * 512:(tb + 1) * 512], data0=ones16[:],
                                                                   data1=spT[:], initial=init,
                                                                   op0=ALU.mult, op1=ALU.add),
                             reads=[SPT], writes=[FN])
                    P.deps("pe", [FN, IDB], PB[3])
                    ins = None
                    for i in range(NT):
                        ins = nc.tensor.transpose(out=PS[3][:, i * 16:(i + 1) * 16], in_=Fneg[:, i * 128:(i + 1) * 128],
                                                  identity=ident[0:16, 0:16])
                    P.cnt["pe"] += 1
                    ins.then_inc(P.sem["pe"], 1)
                    tk = Tk(P.sem["pe"], P.cnt["pe"], "pe")
                    P.mark(tk, [FN, IDB], PB[3])
                    P.op("dve", lambda e: e.tensor_copy(out=Fk[:].rearrange("p i h -> p (i h)"), in_=PS[3][:, 0:256]),
                         reads=PB[3], writes=[FK])

                    fqc = [0]
                    load_qkv(0)
                    for c in range(NCH):
                        wq, wk, wv, WQ = wq2[c % 2], wk2[c % 2], wv2[c % 2], WQ2[c % 2]
                        if c + 1 < NCH:
                            load_qkv(c + 1)
                        for tb in range(4):
                            xin = [XT[tb * 4 + t] for t in range(4)]
                            P.mm(PS[3][:, 0:512], [(wq[:, cc, :], xT[:, cc, tb * 512:(tb + 1) * 512]) for cc in range(NCH)],
                                 reads=[WQ] + xin, writes=[PB[3][0]])
                            P.op("act", lambda e: e.activation(out=qT[:, tb * 512:(tb + 1) * 512], in_=PS[3][:, 0:512],
                                                               func=AF.Copy, scale=0.125), reads=[PB[3][0]], writes=[QK])
                            P.mm(PS[3][:, 512:1024],
                                 [(wk[:, cc, :], xT[:, cc, tb * 512:(tb + 1) * 512]) for cc in range(NCH)],
                                 reads=[WQ] + xin, writes=[PB[3][1]])
                            P.op("dve", lambda e: e.tensor_copy(out=kT[:, tb * 512:(tb + 1) * 512], in_=PS[3][:, 512:1024]),
                                 reads=[PB[3][1]], writes=[QK])
                        for i4 in range(4):
                            bk = i4 % 2
                            rdl = [WQ] + XT[i4 * 4:(i4 + 1) * 4]
                            P.deps("pe", rdl, [PB[3][bk]])
                            ins = None
                            for t in range(4):
                                i = i4 * 4 + t
                                for cc in range(NCH):
                                    ins = nc.tensor.matmul(PS[3][:, bk * 512 + t * 128:bk * 512 + (t + 1) * 128],
                                                           lhsT=xT[:, cc, i * 128:(i + 1) * 128], rhs=wv[:, cc, :],
                                                           start=(cc == 0), stop=(cc == NCH - 1))
                            P.cnt["pe"] += 1
                            ins.then_inc(P.sem["pe"], 1)
                            tk = Tk(P.sem["pe"], P.cnt["pe"], "pe")
                            P.mark(tk, rdl, [PB[3][bk]])
                            P.op("act", lambda e: e.activation(
                                out=vc[:, i4 * 4:(i4 + 1) * 4, :, 0:64],
                                in_=PS[3][:, bk * 512:(bk + 1) * 512].rearrange("p (t h d) -> p t h d", t=4, h=2),
                                func=AF.Copy), reads=[PB[3][bk]], writes=[VC])
                        for hh_ in range(2):
                            h = 2 * c + hh_
                            hb = hh_ * 64
                            sk_ = h % 2
                            P.op("pool", lambda e: e.affine_select(out=sel[sk_][:], in_=neg1[:], pattern=[[0, 128]],
                                                                   compare_op=ALU.is_equal, fill=0.0, base=-h,
                                                                   channel_multiplier=1),
                                 reads=[NEG1], writes=[SEL[sk_]])
                            iters = [(qb, j) for qb in range(4) for j in range(4 * qb + 4)]
                            fqbuf = {}

                            def emit_fq(qb):
                                fk_ = fqc[0] % 2
                                fqc[0] += 1
                                fqbuf[qb] = fk_
                                P.mm(PS[3][:, 0:512], [(sel[sk_][:], Fneg[:, qb * 512:(qb + 1) * 512])],
                                     reads=[SEL[sk_], FN], writes=[PB[3][0]])
                                P.op("act", lambda e: e.activation(out=Fq[fk_][:], in_=PS[3][:, 0:512], func=AF.Copy),
                                     reads=[PB[3][0]], writes=[FQ[fk_]])

                            def geom(n):
                                qb, j = iters[n]
                                c0 = max(0, j - 4 * qb) * 128
                                return qb, j, c0, qb * 512 + c0, (qb + 1) * 512

                            def emit_S(n):
                                qb, j, c0, q0, q1 = geom(n)
                                sps, so, sbuf_ = SPS[n % NS]
                                P.mm(sps[:, so + c0:so + 512],
                                     [(kT[hb:hb + 64, j * 128:(j + 1) * 128], qT[hb:hb + 64, q0:q1])],
                                     reads=[QK], writes=[sbuf_])

                            def emit_soft(n):
                                qb, j, c0, q0, q1 = geom(n)
                                sk = n % NS
                                sps, so, sbuf_ = SPS[sk]
                                fk_ = fqbuf[qb]
                                P.op("dve", lambda e: e.tensor_tensor(out=tmp[sk][:, c0:512],
                                                                      in0=sps[:, so + c0:so + 512],
                                                                      in1=Fq[fk_][:, c0:512], op=ALU.add),
                                     reads=[sbuf_, FQ[fk_]], writes=[TMP[sk]])
                                P.op("act", lambda e: e.activation(out=pt[sk][:, c0:512], in_=tmp[sk][:, c0:512],
                                                                   func=AF.Exp, bias=Fk[:, j, h:h + 1]),
                                     reads=[TMP[sk], FK], writes=[PTB[sk]])
                                if j >= 4 * qb:
                                    P.op("pool", lambda e: e.affine_select(out=pt[sk][:, c0:c0 + 128],
                                                                           in_=pt[sk][:, c0:c0 + 128],
                                                                           pattern=[[1, 128]], compare_op=ALU.is_ge,
                                                                           fill=0.0, base=0, channel_multiplier=-1),
                                         writes=[PTB[sk]])

                            def emit_PV(n):
                                qb, j, c0, q0, q1 = geom(n)
                                sk = n % NS
                                for ii in range(max(0, j - 4 * qb), 4):
                                    i = 4 * qb + ii
                                    ob = 1 + ii // 2
                                    oh = ii % 2
                                    P.deps("pe", [PTB[sk], VC], [PB[ob][oh]] if j == 0 else [])
                                    ins = nc.tensor.matmul(PS[ob][:, oh * 512:oh * 512 + 65],
                                                           lhsT=pt[sk][:, ii * 128:(ii + 1) * 128],
                                                           rhs=vc[:, j, hh_, :], start=(j == 0), stop=(j == i))
                                    P.cnt["pe"] += 1
                                    ins.then_inc(P.sem["pe"], 1)
                                    tk = Tk(P.sem["pe"], P.cnt["pe"], "pe")
                                    P.mark(tk, [PTB[sk], VC], [PB[ob][oh]])

                            def emit_norm(qb):
                                for ii in range(4):
                                    i = 4 * qb + ii
                                    ob = 1 + ii // 2
                                    oh = ii % 2
                                    P.op("dve", lambda e: e.reciprocal(out=rs[:, ii:ii + 1],
                                                                       in_=PS[ob][:, oh * 512 + 64:oh * 512 + 65]),
                                         reads=[PB[ob][oh]], writes=[RS])
                                    P.op("dve", lambda e: e.tensor_scalar(out=otm[:, i, hb:hb + 64],
                                                                          in0=PS[ob][:, oh * 512:oh * 512 + 64],
                                                                          scalar1=rs[:, ii:ii + 1], scalar2=None,
                                                                          op0=ALU.mult),
                                         reads=[PB[ob][oh], RS], writes=[OTM])

                            emit_fq(0)
                            emit_S(0)
                            emit_S(1)
                            emit_S(2)
                            for n in range(len(iters)):
                                qb, j = iters[n]
                                if j == 0 and qb + 1 < 4:
                                    emit_fq(qb + 1)
                                emit_soft(n)
                                if n + 3 < len(iters):
                                    emit_S(n + 3)
                                emit_PV(n)
                                if j == 4 * qb + 3:
                                    emit_norm(qb)
                        for i4 in range(4):
                            bk = i4 % 2
                            P.deps("pe", [OTM, IDB], [PB[3][bk]])
                            ins = None
                            for t in range(4):
                                i = i4 * 4 + t
                                ins = nc.tensor.transpose(out=PS[3][:, bk * 512 + t * 128:bk * 512 + (t + 1) * 128],
                                                          in_=otm[:, i, :], identity=ident[:])
                            P.cnt["pe"] += 1
                            ins.then_inc(P.sem["pe"], 1)
                            tk = Tk(P.sem["pe"], P.cnt["pe"], "pe")
                            P.mark(tk, [OTM, IDB], [PB[3][bk]])
                            P.op("act", lambda e: e.activation(out=oT[:, c, i4 * 512:(i4 + 1) * 512],
                                                               in_=PS[3][:, bk * 512:(bk + 1) * 512], func=AF.Copy),
                                 reads=[PB[3][bk]], writes=[OT])
                    P.barrier()
                with nc.sbuf_tensor(uname("a_wo"), [128, NCH, D], BF16) as wo:
                    WO = Buf()
                    P.dma("pool", d_big, wo[:], dram["fox_w_o"][0].rearrange("(c p) n -> p c n", p=128), writes=[WO])
                    def hmm(i):
                        for half in range(2):
                            P.mm(PS[2][:, half * 512:(half + 1) * 512],
                                 [(oT[:, cc, i * 128:(i + 1) * 128], wo[:, cc, half * 512:(half + 1) * 512])
                                  for cc in range(NCH)], reads=[OT, WO], writes=[PB[2][half]])
                    ln_pipeline(list(range(NT)), hmm, 2, [3, 0], False)
                    P.barrier()

        for stg in plan:
            P.barrier()
            if stg.startswith("lru"):
                j = int(stg[3])
                li = 0 if j == 0 else 3
                lru_stage(j, li, dram["moe_router"][1] if li == 3 else None)
            elif stg == "poolnr":
                pool_stage(1, None)
            elif stg == "lnrouter":
                with ExitStack() as st:
                    def sbl(name, shape, dt):
                        return st.enter_context(nc.sbuf_tensor(uname(name), shape, dt))
                    load_ln(dram["ln_mix_g"][0], dram["ln_mix_b"][0])
                    alloc_router(sbl)
                    load_router(dram["moe_router"][0])
                    ln_pipeline(list(range(NT)), None, None, 3, True)
                    finish_router()
                    P.barrier()
            elif stg.startswith("pool"):
                pool_stage(1, dram["moe_router"][0])
            elif stg.startswith("fox"):
                fox_stage(2)
            elif stg.startswith("ffn"):
                j = int(stg[3])
                li = 0 if j == 0 else 2
                ffn_stage([dram["ffn_w_gu"][j]], [dram["ffn_w_down"][j]], False,
                          dram["ln_ffn_g"][li], dram["ln_ffn_b"][li])
            elif stg.startswith("moe"):
                j = int(stg[3])
                li = 1 if j == 0 else 3
                ffn_stage([dram["moe_w_gu"][j][e_] for e_ in range(NE)], [dram["moe_w_down"][j][e_] for e_ in range(NE)],
                          True, dram["ln_ffn_g"][li], dram["ln_ffn_b"][li])
            elif stg == "ldln":
                load_ln(dram["ln_mix_g"][0], dram["ln_mix_b"][0])
            elif stg == "lnonly":
                load_ln(dram["ln_mix_g"][0], dram["ln_mix_b"][0])
                ln_pipeline(list(range(NT)), None, None, [2, 3], False)
        P.barrier()
        ov = out_d.rearrange("(i p) d -> p i d", p=128)
        for i4 in range(4):
            P.dma("sp", d_out, ov[:, i4 * 4:(i4 + 1) * 4, :], x_tm[:, i4 * 4:(i4 + 1) * 4, :],
                  reads=X[i4 * 4:(i4 + 1) * 4])
        P.barrier()
    return nc


def run(inputs, plan=None, n_cores=8, trace=False):
    plan = FULL_PLAN if plan is None else plan
    nc = build_program(plan)
    x = np.ascontiguousarray(inputs["x"], dtype=np.float32)
    shared = {k: np.ascontiguousarray(inputs[k], dtype=np.float32) for k in INPUT_SHAPES
              if k in needed_inputs(plan)}
    in_maps = []
    for b in range(n_cores):
        m = dict(shared)
        m["x"] = x[b]
        in_maps.append(m)
    res = run_bass_kernel_spmd(nc, in_maps, core_ids=list(range(n_cores)), trace=trace)
    out = np.stack([r["out"] for r in res.results], axis=0)
    return out, res


def kernel(**inputs):
    out, _ = run(inputs)
    return out.astype(np.float32)
```

```python
import numpy as np
from contextlib import ExitStack
import concourse.bass as bass
import concourse.mybir as mybir
from concourse.bass_utils import run_bass_kernel_spmd

F32 = mybir.dt.float32
BF16 = mybir.dt.bfloat16
AF = mybir.ActivationFunctionType
ALU = mybir.AluOpType
AX = mybir.AxisListType

D = 1024
S = 2048
NT = 16
NCH = 8
DFF = 3584
NE = 8
ALPHA = float(8 ** 0.25)
LN_EPS = 1e-5
import os
DBG = {k: True for k in os.environ.get("KDBG", "").split(",") if k}
FULL_PLAN = ["lru0", "ffn0", "pool0", "moe0", "fox0", "ffn1", "lru1", "moe1"]

INPUT_SHAPES = {
    "lru_w_in": [2, 1024, 2048], "lru_conv_w": [2, 4, 1024], "lru_conv_b": [2, 1024],
    "lru_w_gates": [2, 8, 128, 256], "lru_b_gates": [2, 8, 256], "lru_lambda": [2, 1024],
    "lru_w_out": [2, 1024, 1024], "pool_w": [1, 4, 256, 256], "pool_scale": [1, 1024],
    "fox_w_qkvf": [1, 1024, 3088], "fox_b_f": [1, 16], "fox_w_o": [1, 1024, 1024],
    "ffn_w_gu": [2, 1024, 7168], "ffn_w_down": [2, 3584, 1024], "moe_router": [2, 1024, 8],
    "moe_w_gu": [2, 8, 1024, 7168], "moe_w_down": [2, 8, 3584, 1024],
    "ln_mix_g": [4, 1024], "ln_mix_b": [4, 1024], "ln_ffn_g": [4, 1024], "ln_ffn_b": [4, 1024],
}


class Tk:
    __slots__ = ("sem", "val", "eng")

    def __init__(self, sem, val, eng):
        self.sem, self.val, self.eng = sem, val, eng


class Buf:
    __slots__ = ("wts", "rts")

    def __init__(self):
        self.wts = {}
        self.rts = {}

    def add_read(self, tk):
        k = id(tk.sem)
        o = self.rts.get(k)
        if o is None or o.val < tk.val:
            self.rts[k] = tk

    def set_write(self, tk):
        k = id(tk.sem)
        o = self.wts.get(k)
        if o is None or o.val < tk.val:
            self.wts[k] = tk
        self.rts = {}


class Prog:
    def __init__(self, nc, es):
        self.nc = nc
        self.es = es
        self.E = {"pe": nc.tensor, "act": nc.scalar, "dve": nc.vector, "pool": nc.gpsimd, "sp": nc.sync}
        self.sem = {k: es.enter_context(nc.semaphore("s_" + k)) for k in self.E}
        self.cnt = {k: 0 for k in self.E}
        self.waited = {k: {} for k in self.E}
        self.dsems = []
        self.dcnt = {}
        self.dpool = {}
        self.dnext = {}

    def wait(self, eng, tk):
        if tk is None or (tk.eng == eng and eng in ("pe", "sp")):
            return
        w = self.waited[eng]
        k = id(tk.sem)
        if w.get(k, -1) >= tk.val:
            return
        self.E[eng].wait_ge(tk.sem, tk.val)
        w[k] = tk.val

    def deps(self, eng, reads, writes):
        for b in reads:
            for t in b.wts.values():
                self.wait(eng, t)
        for b in writes:
            for t in b.wts.values():
                self.wait(eng, t)
            for t in b.rts.values():
                self.wait(eng, t)

    def mark(self, tk, reads, writes):
        for b in reads:
            b.add_read(tk)
        for b in writes:
            b.set_write(tk)

    def op(self, eng, fn, reads=(), writes=()):
        self.deps(eng, reads, writes)
        ins = fn(self.E[eng])
        self.cnt[eng] += 1
        ins.then_inc(self.sem[eng], 1)
        tk = Tk(self.sem[eng], self.cnt[eng], eng)
        self.mark(tk, reads, writes)
        return tk

    def mm(self, out_ap, pairs, reads, writes):
        self.deps("pe", reads, writes)
        n = len(pairs)
        ins = None
        for i, (l, r) in enumerate(pairs):
            ins = self.nc.tensor.matmul(out_ap, lhsT=l, rhs=r, start=(i == 0), stop=(i == n - 1))
        self.cnt["pe"] += 1
        ins.then_inc(self.sem["pe"], 1)
        tk = Tk(self.sem["pe"], self.cnt["pe"], "pe")
        self.mark(tk, reads, writes)
        return tk

    def new_dsem(self, name):
        return None

    def dma(self, q, dsem, out, in_, reads=(), writes=(), **kw):
        pool = self.dpool.setdefault(q, [])
        if len(pool) < 14:
            s = self.es.enter_context(self.nc.semaphore(f"dq_{q}_{len(pool)}"))
            pool.append(s)
            self.dsems.append(s)
            self.dcnt[id(s)] = 0
            self.dnext[q] = 0
        else:
            s = pool[self.dnext[q] % len(pool)]
            self.dnext[q] += 1
        if self.dcnt[id(s)] > 0:
            self.wait(q, Tk(s, self.dcnt[id(s)], None))
        self.deps(q, reads, writes)
        self.E[q].dma_start(out=out, in_=in_, **kw).then_inc(s, 16)
        self.dcnt[id(s)] += 16
        tk = Tk(s, self.dcnt[id(s)], None)
        self.mark(tk, reads, writes)
        return tk

    def barrier(self):
        for e in self.E:
            for o in self.E:
                if o != e and self.cnt[o] > 0:
                    self.wait(e, Tk(self.sem[o], self.cnt[o], o))
            for s in self.dsems:
                if self.dcnt[id(s)] > 0:
                    self.wait(e, Tk(s, self.dcnt[id(s)], None))


def needed_inputs(plan):
    need = {"ln_mix_g", "ln_mix_b", "ln_ffn_g", "ln_ffn_b"}
    for stg in plan:
        if stg.startswith("lru"):
            need |= {k for k in INPUT_SHAPES if k.startswith("lru_")}
            if stg == "lru1":
                need.add("moe_router")
        elif stg == "poolnr":
            need |= {"pool_w", "pool_scale"}
        elif stg == "lnrouter":
            need |= {"moe_router"}
        elif stg.startswith("pool"):
            need |= {"pool_w", "pool_scale", "moe_router"}
        elif stg.startswith("fox"):
            need |= {k for k in INPUT_SHAPES if k.startswith("fox_")}
        elif stg.startswith("ffn"):
            need |= {"ffn_w_gu", "ffn_w_down"}
        elif stg.startswith("moe"):
            need |= {"moe_w_gu", "moe_w_down"}
    return need


def build_program(plan):
    nc = bass.Bass("TRN2", target_bir_lowering=False)
    dram = {"x": nc.dram_tensor("x", [S, D], F32, kind="ExternalInput").ap()}
    for k, shp in INPUT_SHAPES.items():
        if k in needed_inputs(plan):
            dram[k] = nc.dram_tensor(k, shp, F32, kind="ExternalInput").ap()
    out_d = nc.dram_tensor("out", [S, D], F32, kind="ExternalOutput").ap()

    with ExitStack() as es:
        P = Prog(nc, es)

        def sb(name, shape, dt):
            return es.enter_context(nc.sbuf_tensor(name, shape, dt))

        uid = [0]

        def uname(name):
            uid[0] += 1
            return f"{name}_{uid[0]}"

        x_tm = sb("x_tm", [128, NT, D], F32)
        xT = sb("xT", [128, NCH, S], BF16)
        X = [Buf() for _ in range(NT)]
        XT = [Buf() for _ in range(NT)]
        ident = sb("ident", [128, 128], F32)
        Gt = sb("ln_g", [128, D], F32)
        Bt = sb("ln_b", [128, D], F32)
        GB = Buf()
        gate = sb("gate", [128, NT, NE], F32)
        GATE = Buf()
        RTR = {}

        def alloc_router(sbl):
            RTR["eq1"] = sbl("eq1", [128, NT, NE], F32)
            RTR["eq2"] = sbl("eq2", [128, NT, NE], F32)
            RTR["M1"] = sbl("rm1", [128, NT], F32)
            RTR["M2"] = sbl("rm2", [128, NT], F32)
            RTR["RT"] = Buf()
            RTR["wr32"] = sbl("wr32", [128, NCH, NE], F32)
            RTR["WR"] = Buf()
            RTR["xT32"] = [sbl(f"xT32_{i}", [128, NCH, 128], F32) for i in range(2)]
            RTR["XT32"] = [Buf(), Buf()]
            RTR["rsm"] = [sbl(f"rsm{i}", [128, 24], F32) for i in range(2)]
            RTR["RSM"] = [Buf(), Buf()]
        lnst = [sb(f"lnst{i}", [128, 2, 6], F32) for i in range(2)]
        lnmv = [sb(f"lnmv{i}", [128, 8], F32) for i in range(2)]
        LNS = [Buf(), Buf()]
        LNM = [Buf(), Buf()]

        PS = [es.enter_context(nc.psum_tensor(f"ps{i}", [128, 1024], F32)) for i in range(4)]
        PB = [[Buf(), Buf()] for _ in range(4)]

        d_x = P.new_dsem("d_x")
        d_ln = P.new_dsem("d_ln")
        d_misc = P.new_dsem("d_misc")
        d_w = [P.new_dsem(f"d_w{i}") for i in range(2)]
        d_w2 = [P.new_dsem(f"d_v{i}") for i in range(2)]
        d_big = P.new_dsem("d_big")
        d_out = P.new_dsem("d_out")

        IDB = Buf()
        P.op("pool", lambda e: e.memset(ident[:], 1.0), writes=[IDB])
        P.op("pool", lambda e: e.affine_select(out=ident[:], in_=ident[:], pattern=[[1, 128]],
                                               compare_op=ALU.is_equal, fill=0.0, base=0,
                                               channel_multiplier=-1), writes=[IDB])

        xv = dram["x"].rearrange("(i p) d -> p i d", p=128)
        for i4 in range(4):
            P.dma("sp", d_x, x_tm[:, i4 * 4:(i4 + 1) * 4, :], xv[:, i4 * 4:(i4 + 1) * 4, :],
                  writes=X[i4 * 4:(i4 + 1) * 4])

        def emit_xT(i, tp, with_router=False):
            tpb = PB[tp]
            P.deps("pe", [X[i], IDB], tpb)
            ins = None
            for c in range(NCH):
                ins = nc.tensor.transpose(out=PS[tp][:, c * 128:(c + 1) * 128],
                                          in_=x_tm[:, i, c * 128:(c + 1) * 128], identity=ident[:])
            P.cnt["pe"] += 1
            ins.then_inc(P.sem["pe"], 1)
            tk = Tk(P.sem["pe"], P.cnt["pe"], "pe")
            P.mark(tk, [X[i], IDB], tpb)
            src = PS[tp][:, :].rearrange("p (c t) -> p c t", c=NCH)
            P.op("act", lambda e: e.activation(out=xT[:, :, i * 128:(i + 1) * 128], in_=src, func=AF.Copy),
                 reads=tpb, writes=[XT[i]])
            if with_router:
                k = i % 2
                eq1, eq2, M1, M2, RT = RTR["eq1"], RTR["eq2"], RTR["M1"], RTR["M2"], RTR["RT"]
                wr32, WR, xT32, XT32, rsm, RSM = (RTR["wr32"], RTR["WR"], RTR["xT32"], RTR["XT32"],
                                                  RTR["rsm"], RTR["RSM"])
                if DBG.get("noactcopy"):
                    P.op("dve", lambda e: e.memset(xT32[k][:], 0.5), writes=[XT32[k]])
                else:
                    P.op("act", lambda e: e.activation(out=xT32[k][:], in_=src, func=AF.Copy),
                         reads=tpb, writes=[XT32[k]])
                if DBG.get("nologit"):
                    P.op("dve", lambda e: e.tensor_copy(out=PS[1][:, 0:NE], in_=xT32[k][:, 0, 0:NE]),
                         reads=[XT32[k], WR], writes=[PB[1][0]])
                else:
                    P.mm(PS[1][:, 0:NE], [(xT32[k][:, c, :], wr32[:, c, :]) for c in range(NCH)],
                         reads=[XT32[k], WR], writes=[PB[1][0]])
                lg = rsm[k][:, 0:8]
                l2 = rsm[k][:, 8:16]
                if DBG.get("nodve"):
                    return
                P.op("dve", lambda e: e.tensor_copy(out=lg, in_=PS[1][:, 0:NE]),
                     reads=[PB[1][0]], writes=[RSM[k]])
                P.op("dve", lambda e: e.tensor_reduce(out=M1[:, i:i + 1], in_=lg, axis=AX.X, op=ALU.max),
                     reads=[RSM[k]], writes=[RT])
                P.op("dve", lambda e: e.tensor_scalar(out=eq1[:, i, :], in0=lg, scalar1=M1[:, i:i + 1],
                                                      scalar2=None, op0=ALU.is_equal),
                     reads=[RSM[k]], writes=[RT])
                P.op("dve", lambda e: e.scalar_tensor_tensor(out=l2, in0=eq1[:, i, :], scalar=-1e30, in1=lg,
                                                             op0=ALU.mult, op1=ALU.add),
                     reads=[RT], writes=[RSM[k]])
                P.op("dve", lambda e: e.tensor_reduce(out=M2[:, i:i + 1], in_=l2, axis=AX.X, op=ALU.max),
                     reads=[RSM[k]], writes=[RT])
                P.op("dve", lambda e: e.tensor_scalar(out=eq2[:, i, :], in0=l2, scalar1=M2[:, i:i + 1],
                                                      scalar2=None, op0=ALU.is_equal),
                     reads=[RSM[k]], writes=[RT])

        def finish_router():
            if DBG.get("nofinish"):
                return
            eq1, eq2, M1, M2, RT = RTR["eq1"], RTR["eq2"], RTR["M1"], RTR["M2"], RTR["RT"]
            rsm, RSM = RTR["rsm"], RTR["RSM"]
            dm = rsm[0][:, 0:16]
            g1 = rsm[1][:, 0:16]
            P.op("dve", lambda e: e.tensor_tensor(out=dm, in0=M2[:, :], in1=M1[:, :], op=ALU.subtract),
                 reads=[RT], writes=[RSM[0]])
            P.op("act", lambda e: e.activation(out=dm, in_=dm, func=AF.Sigmoid), writes=[RSM[0]])
            P.op("dve", lambda e: e.tensor_scalar(out=g1, in0=dm, scalar1=-1.0, scalar2=1.0,
                                                  op0=ALU.mult, op1=ALU.add),
                 reads=[RSM[0]], writes=[RSM[1]])
            for i in range(NT):
                P.op("dve", lambda e: e.tensor_scalar(out=gate[:, i, :], in0=eq1[:, i, :],
                                                      scalar1=g1[:, i:i + 1], scalar2=None, op0=ALU.mult),
                     reads=[RT, RSM[1]], writes=[GATE])
                P.op("dve", lambda e: e.scalar_tensor_tensor(out=gate[:, i, :], in0=eq2[:, i, :],
                                                             scalar=dm[:, i:i + 1], in1=gate[:, i, :],
                                                             op0=ALU.mult, op1=ALU.add),
                     reads=[RT, RSM[0]], writes=[GATE])

        def load_ln(g_ap, b_ap):
            P.dma("sp", d_ln, Gt[:], g_ap.partition_broadcast(128), writes=[GB])
            P.dma("sp", d_ln, Bt[:], b_ap.partition_broadcast(128), writes=[GB])

        def load_router(w_ap):
            if DBG.get("nordma"):
                P.op("dve", lambda e: e.memset(RTR["wr32"][:], 0.01), writes=[RTR["WR"]])
            else:
                P.dma("sp", d_misc, RTR["wr32"][:], w_ap.rearrange("(c p) e -> p c e", p=128), writes=[RTR["WR"]])

        def ln_pipeline(tiles, hmm, hsrc, tps, with_router=False):
            tps = tps if isinstance(tps, (list, tuple)) else [tps]
            n = len(tiles)

            def p1(t):
                i = tiles[t]
                k = i % 2
                xt = x_tm[:, i, :]
                if hsrc is not None:
                    hmm(i)
                    P.op("dve", lambda e: e.scalar_tensor_tensor(out=xt, in0=xt, scalar=ALPHA, in1=PS[hsrc][:, :],
                                                                 op0=ALU.mult, op1=ALU.add),
                         reads=PB[hsrc], writes=[X[i]])
                P.op("dve", lambda e: e.bn_stats(out=lnst[k][:, 0, :], in_=x_tm[:, i, 0:512]),
                     reads=[X[i]], writes=[LNS[k]])
                P.op("dve", lambda e: e.bn_stats(out=lnst[k][:, 1, :], in_=x_tm[:, i, 512:1024]),
                     reads=[X[i]], writes=[LNS[k]])
                mv = lnmv[k]
                P.op("dve", lambda e: e.bn_aggr(out=mv[:, 0:2], in_=lnst[k][:].rearrange("p a b -> p (a b)")),
                     reads=[LNS[k]], writes=[LNM[k]])
                P.op("dve", lambda e: e.tensor_scalar(out=mv[:, 2:3], in0=mv[:, 1:2], scalar1=LN_EPS, scalar2=None,
                                                      op0=ALU.add), reads=[LNM[k]], writes=[LNM[k]])
                P.op("act", lambda e: e.activation(out=mv[:, 3:4], in_=mv[:, 2:3], func=AF.Sqrt),
                     reads=[LNM[k]], writes=[LNM[k]])

            def p2(t):
                i = tiles[t]
                k = i % 2
                xt = x_tm[:, i, :]
                mv = lnmv[k]
                P.op("dve", lambda e: e.reciprocal(out=mv[:, 4:5], in_=mv[:, 3:4]), reads=[LNM[k]], writes=[LNM[k]])
                P.op("dve", lambda e: e.tensor_scalar(out=mv[:, 5:6], in0=mv[:, 0:1], scalar1=mv[:, 4:5], scalar2=-1.0,
                                                      op0=ALU.mult, op1=ALU.mult), reads=[LNM[k]], writes=[LNM[k]])
                P.op("act", lambda e: e.activation(out=xt, in_=xt, func=AF.Identity, scale=mv[:, 4:5], bias=mv[:, 5:6]),
                     reads=[LNM[k]], writes=[X[i]])

            def p3(t):
                i = tiles[t]
                xt = x_tm[:, i, :]
                P.op("dve", lambda e: e.tensor_tensor(out=xt, in0=xt, in1=Gt[:], op=ALU.mult),
                     reads=[GB], writes=[X[i]])
                P.op("pool", lambda e: e.tensor_tensor(out=xt, in0=xt, in1=Bt[:], op=ALU.add),
                     reads=[GB], writes=[X[i]])

            def p4(t):
                i = tiles[t]
                emit_xT(i, tps[t % len(tps)], with_router)

            for t in range(n + 3):
                if t < n:
                    p1(t)
                if 0 <= t - 1 < n:
                    p2(t - 1)
                if 0 <= t - 2 < n:
                    p3(t - 2)
                if 0 <= t - 3 < n:
                    p4(t - 3)

        for i in range(NT):
            emit_xT(i, 2 + (i % 2))

        def ffn_stage(wgu_aps, wd_aps, use_gate, g_ap, b_ap, on_done=None):
            with ExitStack() as st:
                def sbl(name, shape, dt):
                    return st.enter_context(nc.sbuf_tensor(uname(name), shape, dt))
                WG = [sbl(f"f_wg{i}", [128, NCH, 512], BF16) for i in range(2)]
                WU = [sbl(f"f_wu{i}", [128, NCH, 512], BF16) for i in range(2)]
                WD = [sbl(f"f_wd{i}", [128, 4, D], BF16) for i in range(2)]
                WGB = [[Buf() for _ in range(4)] for _ in range(2)]
                WDB = [Buf(), Buf()]
                hT = [sbl(f"f_hT{i}", [128, 4, 512], BF16) for i in range(2)]
                HB = [Buf(), Buf()]
                sg = [sbl(f"f_sg{i}", [128, 512], F32) for i in range(2)]
                SG = [Buf(), Buf()]

                load_ln(g_ap, b_ap)
                for i in range(NT):
                    P.op("act", lambda e: e.activation(out=x_tm[:, i, :], in_=x_tm[:, i, :], func=AF.Copy, scale=ALPHA),
                         writes=[X[i]])
                groups = [(e_, G) for e_ in range(len(wgu_aps)) for G in range(7)]

                def load_w(gi):
                    e_, G = groups[gi]
                    k = gi % 2
                    wgu = wgu_aps[e_].rearrange("(c p) n -> p c n", p=128)
                    wdn = wd_aps[e_].rearrange("(j p) n -> p j n", p=128)
                    if gi == 0:
                        for jj in range(4):
                            c0 = G * 512 + jj * 128
                            P.dma("pool", d_w[k], WG[k][:, :, jj * 128:(jj + 1) * 128], wgu[:, :, c0:c0 + 128],
                                  writes=[WGB[k][jj]])
                            P.dma("pool", d_w[k], WU[k][:, :, jj * 128:(jj + 1) * 128],
                                  wgu[:, :, DFF + c0:DFF + c0 + 128], writes=[WGB[k][jj]])
                    else:
                        P.dma("pool", d_w[k], WG[k][:], wgu[:, :, G * 512:(G + 1) * 512], writes=WGB[k])
                        P.dma("pool", d_w[k], WU[k][:], wgu[:, :, DFF + G * 512:DFF + (G + 1) * 512], writes=WGB[k])
                    P.dma("pool", d_w[k], WD[k][:], wdn[:, G * 4:(G + 1) * 4, :], writes=[WDB[k]])

                steps = [(gi, tb) for gi in range(len(groups)) for tb in range(4)]
                cnt = [0]

                def gu(s):
                    gi, tb = steps[s]
                    k = gi % 2
                    hk = s % 2
                    xin = [XT[tb * 4 + t] for t in range(4)]
                    for jj in range(4):
                        q = cnt[0] % 2
                        cnt[0] += 1
                        P.mm(PS[q][:, 0:512],
                             [(WG[k][:, c, jj * 128:(jj + 1) * 128], xT[:, c, tb * 512:(tb + 1) * 512])
                              for c in range(NCH)], reads=[WGB[k][jj]] + xin, writes=[PB[q][0]])
                        P.mm(PS[q][:, 512:1024],
                             [(WU[k][:, c, jj * 128:(jj + 1) * 128], xT[:, c, tb * 512:(tb + 1) * 512])
                              for c in range(NCH)], reads=[WGB[k][jj]] + xin, writes=[PB[q][1]])
                        P.op("act", lambda e: e.activation(out=sg[q][:], in_=PS[q][:, 0:512], func=AF.Silu),
                             reads=[PB[q][0]], writes=[SG[q]])
                        P.op("dve", lambda e: e.tensor_tensor(out=hT[hk][:, jj, :], in0=sg[q][:],
                                                              in1=PS[q][:, 512:1024], op=ALU.mult),
                             reads=[SG[q], PB[q][1]], writes=[HB[hk]])

                def down(s):
                    gi, tb = steps[s]
                    e_, G = groups[gi]
                    k = gi % 2
                    hk = s % 2
                    for tt in range(4):
                        i = tb * 4 + tt
                        y = 2 + (tt % 2)
                        for half in range(2):
                            P.mm(PS[y][:, half * 512:(half + 1) * 512],
                                 [(hT[hk][:, jj, tt * 128:(tt + 1) * 128], WD[k][:, jj, half * 512:(half + 1) * 512])
                                  for jj in range(4)], reads=[WDB[k], HB[hk]], writes=[PB[y][half]])
                        sc = gate[:, i, e_:e_ + 1] if use_gate else 1.0
                        rd = PB[y] + ([GATE] if use_gate else [])
                        P.op("dve", lambda e: e.scalar_tensor_tensor(out=x_tm[:, i, :], in0=PS[y][:, :], scalar=sc,
                                                                     in1=x_tm[:, i, :], op0=ALU.mult, op1=ALU.add),
                             reads=rd, writes=[X[i]])

                def tail_ln(s_done):
                    gi_d, tb_d = steps[s_done]
                    if gi_d == len(groups) - 1:
                        tl_ = [tb_d * 4 + t for t in range(4)]
                        ln_pipeline(tl_, None, None, [2, 3], False)
                        if on_done is not None:
                            on_done(tl_)

                load_w(0)
                for s in range(len(steps)):
                    gi, tb = steps[s]
                    gu(s)
                    if s > 0:
                        down(s - 1)
                        tail_ln(s - 1)
                    if tb == 0 and gi + 1 < len(groups):
                        load_w(gi + 1)
                down(len(steps) - 1)
                tail_ln(len(steps) - 1)
                return

        def ffn_ln():
            ln_pipeline(list(range(NT)), None, None, [2, 3], False)

        def lru_stage(j, li, router_w):
            with ExitStack() as st:
                def sbl(name, shape, dt):
                    return st.enter_context(nc.sbuf_tensor(uname(name), shape, dt))
                praw = sbl("l_praw", [64, 128], F32)
                prm = sbl("l_prm", [128, 64], F32)
                prh = sbl("l_prh", [128, 16], F32)
                nsp = sbl("l_nsp", [128, 8], F32)
                nsph = sbl("l_nsph", [128, 8], F32)
                PRM = Buf()
                wout = sbl("l_wout", [128, NCH, D], BF16)
                WOUT = Buf()
                wgt = sbl("l_wg", [128, 8, 256], BF16)
                WGT = Buf()
                win = sbl("l_win", [128, NCH, 2 * D], BF16)
                WINQ = [Buf() for _ in range(4)]
                mT = [sbl("l_mT0", [128, NCH, 512], BF16)]
                MT = [Buf()]
                halo = sbl("l_halo", [128, 8, 4], F32)
                hlast = sbl("l_hlast", [128, 8], F32)
                HALO = [Buf() for _ in range(8)]
                HL = [Buf() for _ in range(8)]

                def tmp2(name, shape, dt):
                    return [sbl(f"{name}{i}", shape, dt) for i in range(2)], [Buf(), Buf()]
                yg = [sbl(f"l_yg{i}", [128, 512], BF16) for i in range(3)]
                YG = [Buf(), Buf(), Buf()]
                rec, REC = tmp2("l_rec", [128, 516], F32)
                xr, XR = tmp2("l_xr", [128, 512], F32)
                xrb, XRB = tmp2("l_xrb", [128, 512], BF16)
                ra, RA = tmp2("l_ra", [128, 512], F32)
                it, IT = tmp2("l_it", [128, 512], F32)
                mu, MU = tmp2("l_mu", [128, 512], F32)
                hh, HH = tmp2("l_hh", [128, 512], F32)

                load_ln(dram["ln_mix_g"][li], dram["ln_mix_b"][li])
                if router_w is not None:
                    alloc_router(sbl)
                    load_router(router_w)
                PRAW = Buf()
                P.dma("sp", d_misc, praw[0:32, :], dram["lru_conv_w"][j].rearrange("k (c p) -> (k c) p", p=128),
                      writes=[PRAW])
                P.dma("sp", d_misc, praw[32:40, :], dram["lru_conv_b"][j].rearrange("(c p) -> c p", p=128),
                      writes=[PRAW])
                P.dma("sp", d_misc, praw[40:48, :], dram["lru_lambda"][j].rearrange("(c p) -> c p", p=128),
                      writes=[PRAW])
                P.dma("sp", d_misc, praw[48:64, :], dram["lru_b_gates"][j].rearrange("n (h p) -> (n h) p", p=128),
                      writes=[PRAW])
                winv = dram["lru_w_in"][j].rearrange("(c p) n -> p c n", p=128)
                for q4 in (0, 2, 1, 3):
                    P.dma("pool", d_big, win[:, :, q4 * 512:(q4 + 1) * 512], winv[:, :, q4 * 512:(q4 + 1) * 512],
                          writes=[WINQ[q4]])
                P.dma("pool", d_big, wgt[:], dram["lru_w_gates"][j].rearrange("n p g -> p n g"), writes=[WGT])
                P.dma("pool", d_big, wout[:], dram["lru_w_out"][j].rearrange("(c p) n -> p c n", p=128),
                      writes=[WOUT])
                P.deps("pe", [PRAW, IDB], PB[3])
                ins = nc.tensor.transpose(out=PS[3][:, 0:64], in_=praw[:, :], identity=ident[0:64, 0:64])
                P.cnt["pe"] += 1
                ins.then_inc(P.sem["pe"], 1)
                tk = Tk(P.sem["pe"], P.cnt["pe"], "pe")
                P.mark(tk, [PRAW, IDB], PB[3])
                P.op("dve", lambda e: e.tensor_copy(out=prm[:], in_=PS[3][:, 0:64]), reads=PB[3], writes=[PRM])
                P.op("dve", lambda e: e.tensor_scalar(out=prh[:], in0=prm[:, 48:64], scalar1=0.5, scalar2=None,
                                                      op0=ALU.mult), writes=[PRM])
                P.op("act", lambda e: e.activation(out=nsp[:], in_=prm[:, 40:48], func=AF.Exp, scale=-1.0),
                     reads=[PRM], writes=[PRM])
                P.op("act", lambda e: e.activation(out=nsp[:], in_=nsp[:], func=AF.Ln, bias=1.0),
                     reads=[PRM], writes=[PRM])
                P.op("dve", lambda e: e.tensor_scalar(out=nsp[:], in0=nsp[:], scalar1=-8.0, scalar2=None,
                                                      op0=ALU.mult), reads=[PRM], writes=[PRM])
                P.op("dve", lambda e: e.tensor_scalar(out=nsph[:], in0=nsp[:], scalar1=0.5, scalar2=None,
                                                      op0=ALU.mult), writes=[PRM])
                P.op("dve", lambda e: e.memset(halo[:], 0.0), writes=HALO)
                P.op("dve", lambda e: e.memset(hlast[:], 0.0), writes=HL)

                steps = [(tb, c) for tb in range(4) for c in range(8)]

                def mm_gr(s):
                    tb, c = steps[s]
                    q = s % 2
                    xin = [XT[tb * 4 + t] for t in range(4)]
                    t0 = tb * 512
                    P.mm(PS[q][:, 0:512], [(win[:, cc, c * 128:(c + 1) * 128], xT[:, cc, t0:t0 + 512])
                                           for cc in range(NCH)], reads=[WINQ[c // 4]] + xin, writes=[PB[q][0]])
                    P.mm(PS[q][:, 512:1024], [(win[:, cc, D + c * 128:D + (c + 1) * 128], xT[:, cc, t0:t0 + 512])
                                              for cc in range(NCH)], reads=[WINQ[2 + c // 4]] + xin, writes=[PB[q][1]])

                def part_a(s):
                    tb, c = steps[s]
                    k = s % 2
                    q = s % 2
                    P.op("act", lambda e: e.activation(out=yg[s % 3][:], in_=PS[q][:, 0:512], func=AF.Gelu_apprx_tanh),
                         reads=[PB[q][0]], writes=[YG[s % 3]])
                    P.op("pool", lambda e: e.tensor_copy(out=rec[k][:, 0:4], in_=halo[:, c, :]),
                         reads=[HALO[c]], writes=[REC[k]])
                    P.op("dve", lambda e: e.tensor_copy(out=rec[k][:, 4:516], in_=PS[q][:, 512:1024]),
                         reads=[PB[q][1]], writes=[REC[k]])

                def c_dve(s):
                    tb, c = steps[s]
                    k = s % 2
                    q = s % 2
                    P.op("pool", lambda e: e.tensor_copy(out=halo[:, c, :], in_=rec[k][:, 512:516]),
                         reads=[REC[k]], writes=[HALO[c]])
                    P.op("dve", lambda e: e.tensor_scalar(out=xr[k][:], in0=rec[k][:, 4:516],
                                                          scalar1=prm[:, 24 + c:25 + c], scalar2=prm[:, 32 + c:33 + c],
                                                          op0=ALU.mult, op1=ALU.add),
                         reads=[REC[k], PRM], writes=[XR[k]])
                    for kk in range(3):
                        sh = 3 - kk
                        P.op("dve", lambda e: e.scalar_tensor_tensor(out=xr[k][:], in0=rec[k][:, 4 - sh:516 - sh],
                                                                     scalar=prm[:, kk * 8 + c:kk * 8 + c + 1],
                                                                     in1=xr[k][:], op0=ALU.mult, op1=ALU.add),
                             reads=[REC[k], PRM], writes=[XR[k]])

                def c_act(s):
                    tb, c = steps[s]
                    k = s % 2
                    q = s % 2
                    P.op("act", lambda e: e.activation(out=xrb[k][:], in_=xr[k][:], func=AF.Copy),
                         reads=[XR[k]], writes=[XRB[k]])
                    P.mm(PS[q][:, 0:512], [(wgt[:, c, 0:128], xrb[k][:])], reads=[WGT, XRB[k]], writes=[PB[q][0]])
                    P.mm(PS[q][:, 512:1024], [(wgt[:, c, 128:256], xrb[k][:])], reads=[WGT, XRB[k]],
                         writes=[PB[q][1]])

                def e_act(s):
                    tb, c = steps[s]
                    k = s % 2
                    q = s % 2
                    P.op("act", lambda e: e.activation(out=ra[k][:], in_=PS[q][:, 0:512], func=AF.Tanh, scale=0.5,
                                                       bias=prh[:, 2 * c:2 * c + 1]),
                         reads=[PB[q][0], PRM], writes=[RA[k]])
                    P.op("act", lambda e: e.activation(out=it[k][:], in_=PS[q][:, 512:1024], func=AF.Tanh, scale=0.5,
                                                       bias=prh[:, 2 * c + 1:2 * c + 2]),
                         reads=[PB[q][1], PRM], writes=[IT[k]])
                    P.op("act", lambda e: e.activation(out=mu[k][:], in_=ra[k][:], func=AF.Exp, scale=nsp[:, c:c + 1],
                                                       bias=nsp[:, c:c + 1]),
                         reads=[RA[k], PRM], writes=[MU[k]])
                    P.op("act", lambda e: e.activation(out=ra[k][:], in_=ra[k][:], func=AF.Exp, scale=nsph[:, c:c + 1],
                                                       bias=nsph[:, c:c + 1]),
                         reads=[PRM], writes=[RA[k]])
                    P.op("act", lambda e: e.activation(out=mu[k][:], in_=mu[k][:], func=AF.Ln, scale=-1.0, bias=1.0),
                         writes=[MU[k]])
                    P.op("act", lambda e: e.activation(out=mu[k][:], in_=mu[k][:], func=AF.Exp, scale=0.5),
                         writes=[MU[k]])

                def f_dve(s):
                    tb, c = steps[s]
                    k = s % 2
                    q = s % 2
                    P.op("dve", lambda e: e.scalar_tensor_tensor(out=it[k][:], in0=it[k][:], scalar=1.0, in1=xr[k][:],
                                                                 op0=ALU.add, op1=ALU.mult),
                         reads=[XR[k]], writes=[IT[k]])
                    P.op("dve", lambda e: e.scalar_tensor_tensor(out=mu[k][:], in0=mu[k][:], scalar=0.5, in1=it[k][:],
                                                                 op0=ALU.mult, op1=ALU.mult),
                         reads=[IT[k]], writes=[MU[k]])
                    P.op("dve", lambda e: e.tensor_tensor_scan(out=hh[k][:], data0=ra[k][:], data1=mu[k][:],
                                                               initial=hlast[:, c:c + 1], op0=ALU.mult, op1=ALU.add),
                         reads=[RA[k], MU[k], HL[c]], writes=[HH[k]])
                    P.op("dve", lambda e: e.tensor_copy(out=hlast[:, c:c + 1], in_=hh[k][:, 511:512]),
                         reads=[HH[k]], writes=[HL[c]])
                    P.op("pool", lambda e: e.tensor_tensor(out=mT[0][:, c, :], in0=yg[s % 3][:], in1=hh[k][:], op=ALU.mult),
                         reads=[YG[s % 3], HH[k]], writes=[MT[0]])

                mm_gr(0)
                part_a(0)
                c_dve(0)
                c_act(0)
                for s, (tb, c) in enumerate(steps):
                    if s + 1 < len(steps):
                        mm_gr(s + 1)
                        part_a(s + 1)
                        c_dve(s + 1)
                    e_act(s)
                    if s + 1 < len(steps):
                        c_act(s + 1)
                    f_dve(s)
                    if c == 7:
                        tiles = [tb * 4 + tt for tt in range(4)]

                        def hmm(i):
                            tt = i % 4
                            for half in range(2):
                                P.mm(PS[2][:, half * 512:(half + 1) * 512],
                                     [(mT[0][:, cc, tt * 128:(tt + 1) * 128], wout[:, cc, half * 512:(half + 1) * 512])
                                      for cc in range(NCH)], reads=[MT[0], WOUT], writes=[PB[2][half]])
                        ln_pipeline(tiles, hmm, 2, 3, router_w is not None)
                if router_w is not None:
                    finish_router()

        def pool_stage(li, router_w):
            with ExitStack() as st:
                def sbl(name, shape, dt):
                    return st.enter_context(nc.sbuf_tensor(uname(name), shape, dt))
                pT = sbl("p_pT", [128, NCH, S], BF16)
                PT_ = [Buf() for _ in range(NCH)]
                pw32 = sbl("p_w32", [128, 4, 2, 256], F32)
                pwb = sbl("p_wb", [128, 4, 2, 256], BF16)
                scb = sbl("p_scb", [128, D], F32)
                PW = Buf()
                rc16 = sbl("p_rc", [128, 16], F32)
                RC = Buf()
                A = [sbl(f"p_A{i}", [128, S], F32) for i in range(2)]
                Bq = [sbl(f"p_B{i}", [128, S], F32) for i in range(2)]
                AB = [Buf(), Buf()]
                tsm = [sbl(f"p_t{i}", [128, 16], F32) for i in range(2)]

                load_ln(dram["ln_mix_g"][li], dram["ln_mix_b"][li])
                if router_w is not None:
                    alloc_router(sbl)
                    load_router(router_w)
                P.dma("sp", d_misc, pw32[:], dram["pool_w"][0].rearrange("g (k p) d -> p g k d", p=128), writes=[PW])
                P.dma("sp", d_misc, scb[:], dram["pool_scale"][0].partition_broadcast(128), writes=[PW])
                for g in range(4):
                    for kc in range(2):
                        P.op("dve", lambda e: e.tensor_tensor(out=pwb[:, g, kc, :], in0=pw32[:, g, kc, :],
                                                              in1=scb[:, g * 256:(g + 1) * 256], op=ALU.mult),
                             reads=[PW], writes=[PW])
                for t in range(16):
                    P.op("dve", lambda e: e.memset(rc16[:, t:t + 1], 1.0 / (t + 1)), writes=[RC])
                for c in range(NCH):
                    g = c // 2
                    wl = 2 ** (g + 1)
                    k = c % 2
                    eng = "dve"
                    xc = xT[:, c, :]
                    srcb = xc
                    bufs2 = [A[k], Bq[k]]
                    bi = 0
                    sh = 1
                    while sh < wl:
                        dst = bufs2[bi]
                        P.op(eng, lambda e: e.tensor_tensor(out=dst[:, sh:S], in0=srcb[:, sh:S], in1=srcb[:, 0:S - sh],
                                                            op=ALU.add), reads=XT, writes=[AB[k]])
                        P.op(eng, lambda e: e.tensor_copy(out=dst[:, 0:sh], in_=srcb[:, 0:sh]), reads=XT, writes=[AB[k]])
                        srcb = dst
                        bi ^= 1
                        sh *= 2
                    cur = srcb
                    P.op("dve", lambda e: e.scalar_tensor_tensor(out=pT[:, c, :], in0=cur[:, :], scalar=1.0 / wl,
                                                                 in1=xc, op0=ALU.mult, op1=ALU.subtract),
                         reads=[AB[k]] + XT, writes=[PT_[c]])
                    if wl > 2:
                        n = wl - 1
                        P.op("dve", lambda e: e.tensor_tensor(out=tsm[k][:, 0:n], in0=cur[:, 0:n], in1=rc16[:, 0:n],
                                                              op=ALU.mult), reads=[AB[k], RC], writes=[AB[k]])
                        P.op("dve", lambda e: e.tensor_tensor(out=pT[:, c, 0:n], in0=tsm[k][:, 0:n], in1=xT[:, c, 0:n],
                                                              op=ALU.subtract), reads=[AB[k]], writes=[PT_[c]])
                    else:
                        P.op("dve", lambda e: e.memset(pT[:, c, 0:1], 0.0), writes=[PT_[c]])
                def hmm(i):
                    for g in range(4):
                        half = g // 2
                        P.mm(PS[2][:, g * 256:(g + 1) * 256],
                             [(pT[:, 2 * g + kc, i * 128:(i + 1) * 128], pwb[:, g, kc, :]) for kc in range(2)],
                             reads=[PT_[2 * g], PT_[2 * g + 1], PW], writes=[PB[2][half]])
                ln_pipeline(list(range(NT)), hmm, 2, 3, router_w is not None)
                if router_w is not None:
                    finish_router()

        def fox_stage(li):
            wqkvf = dram["fox_w_qkvf"][0].rearrange("(c p) n -> p c n", p=128)
            with ExitStack() as st0:
                oT = st0.enter_context(nc.sbuf_tensor(uname("a_oT"), [128, NCH, S], BF16))
                OT = Buf()
                load_ln(dram["ln_mix_g"][li], dram["ln_mix_b"][li])
                with ExitStack() as st:
                    def sbl(name, shape, dt):
                        return st.enter_context(nc.sbuf_tensor(uname(name), shape, dt))
                    wf = sbl("a_wf", [128, NCH, 16], BF16)
                    WF = Buf()
                    nbf = sbl("a_nbf", [16, 1], F32)
                    NBF = Buf()
                    ones16 = sbl("a_ones", [16, 512], F32)
                    spT = sbl("a_spT", [16, 512], F32)
                    SPT = Buf()
                    Fneg = sbl("a_Fneg", [16, S], F32)
                    FN = Buf()
                    Fk = sbl("a_Fk", [128, NT, 16], F32)
                    FK = Buf()
                    neg1 = sbl("a_neg1", [16, 128], F32)
                    sel = [sbl(f"a_sel{i}", [16, 128], F32) for i in range(2)]
                    SEL = [Buf(), Buf()]
                    NEG1 = Buf()
                    wq2 = [sbl(f"a_wq{i}", [128, NCH, 128], BF16) for i in range(2)]
                    wk2 = [sbl(f"a_wk{i}", [128, NCH, 128], BF16) for i in range(2)]
                    wv2 = [sbl(f"a_wv{i}", [128, NCH, 128], BF16) for i in range(2)]
                    WQ2 = [Buf(), Buf()]

                    def load_qkv(cq):
                        kq = cq % 2
                        P.dma("pool", d_w[0], wq2[kq][:], wqkvf[:, :, cq * 128:(cq + 1) * 128], writes=[WQ2[kq]])
                        P.dma("pool", d_w[0], wk2[kq][:], wqkvf[:, :, D + cq * 128:D + (cq + 1) * 128],
                              writes=[WQ2[kq]])
                        P.dma("pool", d_w[0], wv2[kq][:], wqkvf[:, :, 2 * D + cq * 128:2 * D + (cq + 1) * 128],
                              writes=[WQ2[kq]])
                    qT = sbl("a_qT", [128, S], BF16)
                    kT = sbl("a_kT", [128, S], BF16)
                    QK = Buf()
                    vc = sbl("a_vc", [128, NT, 2, 65], BF16)
                    VC = Buf()
                    Fq = [sbl(f"a_Fq{i}", [128, 512], F32) for i in range(2)]
                    FQ = [Buf(), Buf()]
                    tmp = [sbl(f"a_tmp{i}", [128, 512], F32) for i in range(4)]
                    TMP = [Buf() for _ in range(4)]
                    pt = [sbl(f"a_pt{i}", [128, 512], BF16) for i in range(4)]
                    PTB = [Buf() for _ in range(4)]
                    SPS = [(PS[0], 0, PB[0][0]), (PS[0], 512, PB[0][1]), (PS[3], 512, PB[3][1]), (PS[3], 0, PB[3][0])]
                    NS = len(SPS)
                    otm = sbl("a_otm", [128, NT, 128], F32)
                    OTM = Buf()
                    rs = sbl("a_rs", [128, 8], F32)
                    RS = Buf()

                    P.dma("pool", d_big, wf[:], wqkvf[:, :, 3 * D:3 * D + 16], writes=[WF])
                    P.dma("sp", d_misc, nbf[:], dram["fox_b_f"][0].rearrange("(h o) -> h o", o=1), writes=[NBF])
                    P.op("dve", lambda e: e.tensor_scalar(out=nbf[:], in0=nbf[:], scalar1=-1.0, scalar2=None,
                                                          op0=ALU.mult), writes=[NBF])
                    P.op("pool", lambda e: e.memset(ones16[:], 1.0), writes=[SPT])
                    P.op("pool", lambda e: e.memset(neg1[:], -1.0), writes=[NEG1])
                    P.op("pool", lambda e: e.memset(vc[:], 1.0), writes=[VC])
                    for tb in range(4):
                        xin = [XT[tb * 4 + t] for t in range(4)]
                        P.mm(PS[3][0:16, 0:512], [(wf[:, c, :], xT[:, c, tb * 512:(tb + 1) * 512]) for c in range(NCH)],
                             reads=[WF] + xin, writes=[PB[3][0]])
                        P.op("act", lambda e: e.activation(out=spT[:], in_=PS[3][0:16, 0:512],
                                                           func=AF.Exp, scale=-1.0, bias=nbf[:, 0:1]),
                             reads=[PB[3][0], NBF], writes=[SPT])
                        P.op("act", lambda e: e.activation(out=spT[:], in_=spT[:], func=AF.Ln, bias=1.0), writes=[SPT])
                        init = 0.0 if tb == 0 else Fneg[:, tb * 512 - 1:tb * 512]
                        P.op("dve", lambda e: e.tensor_tensor_scan(out=Fneg[:, tb * 512:(tb + 1) * 512], data0=ones16[:],
                                                                   data1=spT[:], initial=init,
                                                                   op0=ALU.mult, op1=ALU.add),
                             reads=[SPT], writes=[FN])
                    P.deps("pe", [FN, IDB], PB[3])
                    ins = None
                    for i in range(NT):
                        ins = nc.tensor.transpose(out=PS[3][:, i * 16:(i + 1) * 16], in_=Fneg[:, i * 128:(i + 1) * 128],
                                                  identity=ident[0:16, 0:16])
                    P.cnt["pe"] += 1
                    ins.then_inc(P.sem["pe"], 1)
                    tk = Tk(P.sem["pe"], P.cnt["pe"], "pe")
                    P.mark(tk, [FN, IDB], PB[3])
                    P.op("dve", lambda e: e.tensor_copy(out=Fk[:].rearrange("p i h -> p (i h)"), in_=PS[3][:, 0:256]),
                         reads=PB[3], writes=[FK])

                    fqc = [0]
                    load_qkv(0)
                    for c in range(NCH):
                        wq, wk, wv, WQ = wq2[c % 2], wk2[c % 2], wv2[c % 2], WQ2[c % 2]
                        if c + 1 < NCH:
                            load_qkv(c + 1)
                        for tb in range(4):
                            xin = [XT[tb * 4 + t] for t in range(4)]
                            P.mm(PS[3][:, 0:512], [(wq[:, cc, :], xT[:, cc, tb * 512:(tb + 1) * 512]) for cc in range(NCH)],
                                 reads=[WQ] + xin, writes=[PB[3][0]])
                            P.op("act", lambda e: e.activation(out=qT[:, tb * 512:(tb + 1) * 512], in_=PS[3][:, 0:512],
                                                               func=AF.Copy, scale=0.125), reads=[PB[3][0]], writes=[QK])
                            P.mm(PS[3][:, 512:1024],
                                 [(wk[:, cc, :], xT[:, cc, tb * 512:(tb + 1) * 512]) for cc in range(NCH)],
                                 reads=[WQ] + xin, writes=[PB[3][1]])
                            P.op("dve", lambda e: e.tensor_copy(out=kT[:, tb * 512:(tb + 1) * 512], in_=PS[3][:, 512:1024]),
                                 reads=[PB[3][1]], writes=[QK])
                        for i4 in range(4):
                            bk = i4 % 2
                            rdl = [WQ] + XT[i4 * 4:(i4 + 1) * 4]
                            P.deps("pe", rdl, [PB[3][bk]])
                            ins = None
                            for t in range(4):
                                i = i4 * 4 + t
                                for cc in range(NCH):
                                    ins = nc.tensor.matmul(PS[3][:, bk * 512 + t * 128:bk * 512 + (t + 1) * 128],
                                                           lhsT=xT[:, cc, i * 128:(i + 1) * 128], rhs=wv[:, cc, :],
                                                           start=(cc == 0), stop=(cc == NCH - 1))
                            P.cnt["pe"] += 1
                            ins.then_inc(P.sem["pe"], 1)
                            tk = Tk(P.sem["pe"], P.cnt["pe"], "pe")
                            P.mark(tk, rdl, [PB[3][bk]])
                            P.op("act", lambda e: e.activation(
                                out=vc[:, i4 * 4:(i4 + 1) * 4, :, 0:64],
                                in_=PS[3][:, bk * 512:(bk + 1) * 512].rearrange("p (t h d) -> p t h d", t=4, h=2),
                                func=AF.Copy), reads=[PB[3][bk]], writes=[VC])
                        for hh_ in range(2):
                            h = 2 * c + hh_
                            hb = hh_ * 64
                            sk_ = h % 2
                            P.op("pool", lambda e: e.affine_select(out=sel[sk_][:], in_=neg1[:], pattern=[[0, 128]],
                                                                   compare_op=ALU.is_equal, fill=0.0, base=-h,
                                                                   channel_multiplier=1),
                                 reads=[NEG1], writes=[SEL[sk_]])
                            iters = [(qb, j) for qb in range(4) for j in range(4 * qb + 4)]
                            fqbuf = {}

                            def emit_fq(qb):
                                fk_ = fqc[0] % 2
                                fqc[0] += 1
                                fqbuf[qb] = fk_
                                P.mm(PS[3][:, 0:512], [(sel[sk_][:], Fneg[:, qb * 512:(qb + 1) * 512])],
                                     reads=[SEL[sk_], FN], writes=[PB[3][0]])
                                P.op("act", lambda e: e.activation(out=Fq[fk_][:], in_=PS[3][:, 0:512], func=AF.Copy),
                                     reads=[PB[3][0]], writes=[FQ[fk_]])

                            def geom(n):
                                qb, j = iters[n]
                                c0 = max(0, j - 4 * qb) * 128
                                return qb, j, c0, qb * 512 + c0, (qb + 1) * 512

                            def emit_S(n):
                                qb, j, c0, q0, q1 = geom(n)
                                sps, so, sbuf_ = SPS[n % NS]
                                P.mm(sps[:, so + c0:so + 512],
                                     [(kT[hb:hb + 64, j * 128:(j + 1) * 128], qT[hb:hb + 64, q0:q1])],
                                     reads=[QK], writes=[sbuf_])

                            def emit_soft(n):
                                qb, j, c0, q0, q1 = geom(n)
                                sk = n % NS
                                sps, so, sbuf_ = SPS[sk]
                                fk_ = fqbuf[qb]
                                P.op("dve", lambda e: e.tensor_tensor(out=tmp[sk][:, c0:512],
                                                                      in0=sps[:, so + c0:so + 512],
                                                                      in1=Fq[fk_][:, c0:512], op=ALU.add),
                                     reads=[sbuf_, FQ[fk_]], writes=[TMP[sk]])
                                P.op("act", lambda e: e.activation(out=pt[sk][:, c0:512], in_=tmp[sk][:, c0:512],
                                                                   func=AF.Exp, bias=Fk[:, j, h:h + 1]),
                                     reads=[TMP[sk], FK], writes=[PTB[sk]])
                                if j >= 4 * qb:
                                    P.op("pool", lambda e: e.affine_select(out=pt[sk][:, c0:c0 + 128],
                                                                           in_=pt[sk][:, c0:c0 + 128],
                                                                           pattern=[[1, 128]], compare_op=ALU.is_ge,
                                                                           fill=0.0, base=0, channel_multiplier=-1),
                                         writes=[PTB[sk]])

                            def emit_PV(n):
                                qb, j, c0, q0, q1 = geom(n)
                                sk = n % NS
                                for ii in range(max(0, j - 4 * qb), 4):
                                    i = 4 * qb + ii
                                    ob = 1 + ii // 2
                                    oh = ii % 2
                                    P.deps("pe", [PTB[sk], VC], [PB[ob][oh]] if j == 0 else [])
                                    ins = nc.tensor.matmul(PS[ob][:, oh * 512:oh * 512 + 65],
                                                           lhsT=pt[sk][:, ii * 128:(ii + 1) * 128],
                                                           rhs=vc[:, j, hh_, :], start=(j == 0), stop=(j == i))
                                    P.cnt["pe"] += 1
                                    ins.then_inc(P.sem["pe"], 1)
                                    tk = Tk(P.sem["pe"], P.cnt["pe"], "pe")
                                    P.mark(tk, [PTB[sk], VC], [PB[ob][oh]])

                            def emit_norm(qb):
                                for ii in range(4):
                                    i = 4 * qb + ii
                                    ob = 1 + ii // 2
                                    oh = ii % 2
                                    P.op("dve", lambda e: e.reciprocal(out=rs[:, ii:ii + 1],
                                                                       in_=PS[ob][:, oh * 512 + 64:oh * 512 + 65]),
                                         reads=[PB[ob][oh]], writes=[RS])
                                    P.op("dve", lambda e: e.tensor_scalar(out=otm[:, i, hb:hb + 64],
                                                                          in0=PS[ob][:, oh * 512:oh * 512 + 64],
                                                                          scalar1=rs[:, ii:ii + 1], scalar2=None,
                                                                          op0=ALU.mult),
                                         reads=[PB[ob][oh], RS], writes=[OTM])

                            emit_fq(0)
                            emit_S(0)
                            emit_S(1)
                            emit_S(2)
                            for n in range(len(iters)):
                                qb, j = iters[n]
                                if j == 0 and qb + 1 < 4:
                                    emit_fq(qb + 1)
                                emit_soft(n)
                                if n + 3 < len(iters):
                                    emit_S(n + 3)
                                emit_PV(n)
                                if j == 4 * qb + 3:
                                    emit_norm(qb)
                        for i4 in range(4):
                            bk = i4 % 2
                            P.deps("pe", [OTM, IDB], [PB[3][bk]])
                            ins = None
                            for t in range(4):
                                i = i4 * 4 + t
                                ins = nc.tensor.transpose(out=PS[3][:, bk * 512 + t * 128:bk * 512 + (t + 1) * 128],
                                                          in_=otm[:, i, :], identity=ident[:])
                            P.cnt["pe"] += 1
                            ins.then_inc(P.sem["pe"], 1)
                            tk = Tk(P.sem["pe"], P.cnt["pe"], "pe")
                            P.mark(tk, [OTM, IDB], [PB[3][bk]])
                            P.op("act", lambda e: e.activation(out=oT[:, c, i4 * 512:(i4 + 1) * 512],
                                                               in_=PS[3][:, bk * 512:(bk + 1) * 512], func=AF.Copy),
                                 reads=[PB[3][bk]], writes=[OT])
                    P.barrier()
                with nc.sbuf_tensor(uname("a_wo"), [128, NCH, D], BF16) as wo:
                    WO = Buf()
                    P.dma("pool", d_big, wo[:], dram["fox_w_o"][0].rearrange("(c p) n -> p c n", p=128), writes=[WO])
                    def hmm(i):
                        for half in range(2):
                            P.mm(PS[2][:, half * 512:(half + 1) * 512],
                                 [(oT[:, cc, i * 128:(i + 1) * 128], wo[:, cc, half * 512:(half + 1) * 512])
                                  for cc in range(NCH)], reads=[OT, WO], writes=[PB[2][half]])
                    ln_pipeline(list(range(NT)), hmm, 2, [3, 0], False)
                    P.barrier()

        out_streamed = [False]
        ov = out_d.rearrange("(i p) d -> p i d", p=128)

        def stream_out(tiles):
            t0_, t1_ = tiles[0], tiles[-1] + 1
            P.dma("sp", d_out, ov[:, t0_:t1_, :], x_tm[:, t0_:t1_, :], reads=X[t0_:t1_])
            out_streamed[0] = True

        for si_, stg in enumerate(plan):
            if si_ > 0:
                P.barrier()
            last_stage = (si_ == len(plan) - 1)
            if stg.startswith("lru"):
                j = int(stg[3])
                li = 0 if j == 0 else 3
                lru_stage(j, li, dram["moe_router"][1] if li == 3 else None)
            elif stg == "poolnr":
                pool_stage(1, None)
            elif stg == "lnrouter":
                with ExitStack() as st:
                    def sbl(name, shape, dt):
                        return st.enter_context(nc.sbuf_tensor(uname(name), shape, dt))
                    load_ln(dram["ln_mix_g"][0], dram["ln_mix_b"][0])
                    alloc_router(sbl)
                    load_router(dram["moe_router"][0])
                    ln_pipeline(list(range(NT)), None, None, 3, True)
                    finish_router()
                    P.barrier()
            elif stg.startswith("pool"):
                pool_stage(1, dram["moe_router"][0])
            elif stg.startswith("fox"):
                fox_stage(2)
            elif stg.startswith("ffn"):
                j = int(stg[3])
                li = 0 if j == 0 else 2
                ffn_stage([dram["ffn_w_gu"][j]], [dram["ffn_w_down"][j]], False,
                          dram["ln_ffn_g"][li], dram["ln_ffn_b"][li], stream_out if last_stage else None)
            elif stg.startswith("moe"):
                j = int(stg[3])
                li = 1 if j == 0 else 3
                ffn_stage([dram["moe_w_gu"][j][e_] for e_ in range(NE)], [dram["moe_w_down"][j][e_] for e_ in range(NE)],
                          True, dram["ln_ffn_g"][li], dram["ln_ffn_b"][li], stream_out if last_stage else None)
            elif stg == "ldln":
                load_ln(dram["ln_mix_g"][0], dram["ln_mix_b"][0])
            elif stg == "lnonly":
                load_ln(dram["ln_mix_g"][0], dram["ln_mix_b"][0])
                ln_pipeline(list(range(NT)), None, None, [2, 3], False)
        P.barrier()
        if not out_streamed[0]:
            for i4 in range(4):
                P.dma("sp", d_out, ov[:, i4 * 4:(i4 + 1) * 4, :], x_tm[:, i4 * 4:(i4 + 1) * 4, :],
                      reads=X[i4 * 4:(i4 + 1) * 4])
        P.barrier()
    return nc


def run(inputs, plan=None, n_cores=8, trace=False):
    plan = FULL_PLAN if plan is None else plan
    nc = build_program(plan)
    x = np.ascontiguousarray(inputs["x"], dtype=np.float32)
    shared = {k: np.ascontiguousarray(inputs[k], dtype=np.float32) for k in INPUT_SHAPES
              if k in needed_inputs(plan)}
    in_maps = []
    for b in range(n_cores):
        m = dict(shared)
        m["x"] = x[b]
        in_maps.append(m)
    res = run_bass_kernel_spmd(nc, in_maps, core_ids=list(range(n_cores)), trace=trace)
    out = np.stack([r["out"] for r in res.results], axis=0)
    return out, res


def kernel(**inputs):
    out, _ = run(inputs)
    return out.astype(np.float32)
```

```python
import numpy as np
from contextlib import ExitStack
import concourse.bass as bass
import concourse.mybir as mybir
from concourse.bass_utils import run_bass_kernel_spmd

F32 = mybir.dt.float32
BF16 = mybir.dt.bfloat16
AF = mybir.ActivationFunctionType
ALU = mybir.AluOpType
AX = mybir.AxisListType

D = 1024
S = 2048
NT = 16
NCH = 8
DFF = 3584
NE = 8
ALPHA = float(8 ** 0.25)
LN_EPS = 1e-5
import os
DBG = {k: True for k in os.environ.get("KDBG", "").split(",") if k}
FULL_PLAN = ["lru0", "ffn0", "pool0", "moe0", "fox0", "ffn1", "lru1", "moe1"]

INPUT_SHAPES = {
    "lru_w_in": [2, 1024, 2048], "lru_conv_w": [2, 4, 1024], "lru_conv_b": [2, 1024],
    "lru_w_gates": [2, 8, 128, 256], "lru_b_gates": [2, 8, 256], "lru_lambda": [2, 1024],
    "lru_w_out": [2, 1024, 1024], "pool_w": [1, 4, 256, 256], "pool_scale": [1, 1024],
    "fox_w_qkvf": [1, 1024, 3088], "fox_b_f": [1, 16], "fox_w_o": [1, 1024, 1024],
    "ffn_w_gu": [2, 1024, 7168], "ffn_w_down": [2, 3584, 1024], "moe_router": [2, 1024, 8],
    "moe_w_gu": [2, 8, 1024, 7168], "moe_w_down": [2, 8, 3584, 1024],
    "ln_mix_g": [4, 1024], "ln_mix_b": [4, 1024], "ln_ffn_g": [4, 1024], "ln_ffn_b": [4, 1024],
}


class Tk:
    __slots__ = ("sem", "val", "eng")

    def __init__(self, sem, val, eng):
        self.sem, self.val, self.eng = sem, val, eng


class Buf:
    __slots__ = ("wts", "rts")

    def __init__(self):
        self.wts = {}
        self.rts = {}

    def add_read(self, tk):
        k = id(tk.sem)
        o = self.rts.get(k)
        if o is None or o.val < tk.val:
            self.rts[k] = tk

    def set_write(self, tk):
        k = id(tk.sem)
        o = self.wts.get(k)
        if o is None or o.val < tk.val:
            self.wts[k] = tk
        self.rts = {}


class Prog:
    def __init__(self, nc, es):
        self.nc = nc
        self.es = es
        self.E = {"pe": nc.tensor, "act": nc.scalar, "dve": nc.vector, "pool": nc.gpsimd, "sp": nc.sync}
        self.sem = {k: es.enter_context(nc.semaphore("s_" + k)) for k in self.E}
        self.cnt = {k: 0 for k in self.E}
        self.waited = {k: {} for k in self.E}
        self.dsems = []
        self.dcnt = {}
        self.dpool = {}
        self.dnext = {}

    def wait(self, eng, tk):
        if tk is None or (tk.eng == eng and eng in ("pe", "sp")):
            return
        w = self.waited[eng]
        k = id(tk.sem)
        if w.get(k, -1) >= tk.val:
            return
        self.E[eng].wait_ge(tk.sem, tk.val)
        w[k] = tk.val

    def deps(self, eng, reads, writes):
        for b in reads:
            for t in b.wts.values():
                self.wait(eng, t)
        for b in writes:
            for t in b.wts.values():
                self.wait(eng, t)
            for t in b.rts.values():
                self.wait(eng, t)

    def mark(self, tk, reads, writes):
        for b in reads:
            b.add_read(tk)
        for b in writes:
            b.set_write(tk)

    def op(self, eng, fn, reads=(), writes=()):
        self.deps(eng, reads, writes)
        ins = fn(self.E[eng])
        self.cnt[eng] += 1
        ins.then_inc(self.sem[eng], 1)
        tk = Tk(self.sem[eng], self.cnt[eng], eng)
        self.mark(tk, reads, writes)
        return tk

    def mm(self, out_ap, pairs, reads, writes):
        self.deps("pe", reads, writes)
        n = len(pairs)
        ins = None
        for i, (l, r) in enumerate(pairs):
            ins = self.nc.tensor.matmul(out_ap, lhsT=l, rhs=r, start=(i == 0), stop=(i == n - 1))
        self.cnt["pe"] += 1
        ins.then_inc(self.sem["pe"], 1)
        tk = Tk(self.sem["pe"], self.cnt["pe"], "pe")
        self.mark(tk, reads, writes)
        return tk

    def new_dsem(self, name):
        return None

    def dma(self, q, dsem, out, in_, reads=(), writes=(), **kw):
        pool = self.dpool.setdefault(q, [])
        if len(pool) < 14:
            s = self.es.enter_context(self.nc.semaphore(f"dq_{q}_{len(pool)}"))
            pool.append(s)
            self.dsems.append(s)
            self.dcnt[id(s)] = 0
            self.dnext[q] = 0
        else:
            s = pool[self.dnext[q] % len(pool)]
            self.dnext[q] += 1
        if self.dcnt[id(s)] > 0:
            self.wait(q, Tk(s, self.dcnt[id(s)], None))
        self.deps(q, reads, writes)
        self.E[q].dma_start(out=out, in_=in_, **kw).then_inc(s, 16)
        self.dcnt[id(s)] += 16
        tk = Tk(s, self.dcnt[id(s)], None)
        self.mark(tk, reads, writes)
        return tk

    def barrier(self):
        for e in self.E:
            for o in self.E:
                if o != e and self.cnt[o] > 0:
                    self.wait(e, Tk(self.sem[o], self.cnt[o], o))
            for s in self.dsems:
                if self.dcnt[id(s)] > 0:
                    self.wait(e, Tk(s, self.dcnt[id(s)], None))


def needed_inputs(plan):
    need = {"ln_mix_g", "ln_mix_b", "ln_ffn_g", "ln_ffn_b"}
    for stg in plan:
        if stg.startswith("lru"):
            need |= {k for k in INPUT_SHAPES if k.startswith("lru_")}
            if stg == "lru1":
                need.add("moe_router")
        elif stg == "poolnr":
            need |= {"pool_w", "pool_scale"}
        elif stg == "lnrouter":
            need |= {"moe_router"}
        elif stg.startswith("pool"):
            need |= {"pool_w", "pool_scale", "moe_router"}
        elif stg.startswith("fox"):
            need |= {k for k in INPUT_SHAPES if k.startswith("fox_")}
        elif stg.startswith("ffn"):
            need |= {"ffn_w_gu", "ffn_w_down"}
        elif stg.startswith("moe"):
            need |= {"moe_w_gu", "moe_w_down"}
    return need


def build_program(plan):
    nc = bass.Bass("TRN2", target_bir_lowering=False)
    dram = {"x": nc.dram_tensor("x", [S, D], F32, kind="ExternalInput").ap()}
    for k, shp in INPUT_SHAPES.items():
        if k in needed_inputs(plan):
            dram[k] = nc.dram_tensor(k, shp, F32, kind="ExternalInput").ap()
    out_d = nc.dram_tensor("out", [S, D], F32, kind="ExternalOutput").ap()

    with ExitStack() as es:
        P = Prog(nc, es)

        def sb(name, shape, dt):
            return es.enter_context(nc.sbuf_tensor(name, shape, dt))

        uid = [0]

        def uname(name):
            uid[0] += 1
            return f"{name}_{uid[0]}"

        x_tm = sb("x_tm", [128, NT, D], F32)
        xT = sb("xT", [128, NCH, S], BF16)
        X = [Buf() for _ in range(NT)]
        XT = [Buf() for _ in range(NT)]
        ident = sb("ident", [128, 128], F32)
        Gt = sb("ln_g", [128, D], F32)
        Bt = sb("ln_b", [128, D], F32)
        GB = Buf()
        gate = sb("gate", [128, NT, NE], F32)
        GATE = Buf()
        RTR = {}

        def alloc_router(sbl):
            RTR["eq1"] = sbl("eq1", [128, NT, NE], F32)
            RTR["eq2"] = sbl("eq2", [128, NT, NE], F32)
            RTR["M1"] = sbl("rm1", [128, NT], F32)
            RTR["M2"] = sbl("rm2", [128, NT], F32)
            RTR["RT"] = Buf()
            RTR["wr32"] = sbl("wr32", [128, NCH, NE], F32)
            RTR["WR"] = Buf()
            RTR["xT32"] = [sbl(f"xT32_{i}", [128, NCH, 128], F32) for i in range(2)]
            RTR["XT32"] = [Buf(), Buf()]
            RTR["rsm"] = [sbl(f"rsm{i}", [128, 24], F32) for i in range(2)]
            RTR["RSM"] = [Buf(), Buf()]
        lnst = [sb(f"lnst{i}", [128, 2, 6], F32) for i in range(2)]
        lnmv = [sb(f"lnmv{i}", [128, 8], F32) for i in range(2)]
        LNS = [Buf(), Buf()]
        LNM = [Buf(), Buf()]

        PS = [es.enter_context(nc.psum_tensor(f"ps{i}", [128, 1024], F32)) for i in range(4)]
        PB = [[Buf(), Buf()] for _ in range(4)]

        d_x = P.new_dsem("d_x")
        d_ln = P.new_dsem("d_ln")
        d_misc = P.new_dsem("d_misc")
        d_w = [P.new_dsem(f"d_w{i}") for i in range(2)]
        d_w2 = [P.new_dsem(f"d_v{i}") for i in range(2)]
        d_big = P.new_dsem("d_big")
        d_out = P.new_dsem("d_out")

        IDB = Buf()
        P.op("pool", lambda e: e.memset(ident[:], 1.0), writes=[IDB])
        P.op("pool", lambda e: e.affine_select(out=ident[:], in_=ident[:], pattern=[[1, 128]],
                                               compare_op=ALU.is_equal, fill=0.0, base=0,
                                               channel_multiplier=-1), writes=[IDB])

        xv = dram["x"].rearrange("(i p) d -> p i d", p=128)
        for i4 in range(4):
            P.dma("sp", d_x, x_tm[:, i4 * 4:(i4 + 1) * 4, :], xv[:, i4 * 4:(i4 + 1) * 4, :],
                  writes=X[i4 * 4:(i4 + 1) * 4])

        def emit_xT(i, tp, with_router=False):
            tpb = PB[tp]
            P.deps("pe", [X[i], IDB], tpb)
            ins = None
            for c in range(NCH):
                ins = nc.tensor.transpose(out=PS[tp][:, c * 128:(c + 1) * 128],
                                          in_=x_tm[:, i, c * 128:(c + 1) * 128], identity=ident[:])
            P.cnt["pe"] += 1
            ins.then_inc(P.sem["pe"], 1)
            tk = Tk(P.sem["pe"], P.cnt["pe"], "pe")
            P.mark(tk, [X[i], IDB], tpb)
            src = PS[tp][:, :].rearrange("p (c t) -> p c t", c=NCH)
            P.op("act", lambda e: e.activation(out=xT[:, :, i * 128:(i + 1) * 128], in_=src, func=AF.Copy),
                 reads=tpb, writes=[XT[i]])
            if with_router:
                k = i % 2
                eq1, eq2, M1, M2, RT = RTR["eq1"], RTR["eq2"], RTR["M1"], RTR["M2"], RTR["RT"]
                wr32, WR, xT32, XT32, rsm, RSM = (RTR["wr32"], RTR["WR"], RTR["xT32"], RTR["XT32"],
                                                  RTR["rsm"], RTR["RSM"])
                if DBG.get("noactcopy"):
                    P.op("dve", lambda e: e.memset(xT32[k][:], 0.5), writes=[XT32[k]])
                else:
                    P.op("act", lambda e: e.activation(out=xT32[k][:], in_=src, func=AF.Copy),
                         reads=tpb, writes=[XT32[k]])
                if DBG.get("nologit"):
                    P.op("dve", lambda e: e.tensor_copy(out=PS[1][:, 0:NE], in_=xT32[k][:, 0, 0:NE]),
                         reads=[XT32[k], WR], writes=[PB[1][0]])
                else:
                    P.mm(PS[1][:, 0:NE], [(xT32[k][:, c, :], wr32[:, c, :]) for c in range(NCH)],
                         reads=[XT32[k], WR], writes=[PB[1][0]])
                lg = rsm[k][:, 0:8]
                l2 = rsm[k][:, 8:16]
                if DBG.get("nodve"):
                    return
                P.op("dve", lambda e: e.tensor_copy(out=lg, in_=PS[1][:, 0:NE]),
                     reads=[PB[1][0]], writes=[RSM[k]])
                P.op("dve", lambda e: e.tensor_reduce(out=M1[:, i:i + 1], in_=lg, axis=AX.X, op=ALU.max),
                     reads=[RSM[k]], writes=[RT])
                P.op("dve", lambda e: e.tensor_scalar(out=eq1[:, i, :], in0=lg, scalar1=M1[:, i:i + 1],
                                                      scalar2=None, op0=ALU.is_equal),
                     reads=[RSM[k]], writes=[RT])
                P.op("dve", lambda e: e.scalar_tensor_tensor(out=l2, in0=eq1[:, i, :], scalar=-1e30, in1=lg,
                                                             op0=ALU.mult, op1=ALU.add),
                     reads=[RT], writes=[RSM[k]])
                P.op("dve", lambda e: e.tensor_reduce(out=M2[:, i:i + 1], in_=l2, axis=AX.X, op=ALU.max),
                     reads=[RSM[k]], writes=[RT])
                P.op("dve", lambda e: e.tensor_scalar(out=eq2[:, i, :], in0=l2, scalar1=M2[:, i:i + 1],
                                                      scalar2=None, op0=ALU.is_equal),
                     reads=[RSM[k]], writes=[RT])

        def finish_router():
            if DBG.get("nofinish"):
                return
            eq1, eq2, M1, M2, RT = RTR["eq1"], RTR["eq2"], RTR["M1"], RTR["M2"], RTR["RT"]
            rsm, RSM = RTR["rsm"], RTR["RSM"]
            dm = rsm[0][:, 0:16]
            g1 = rsm[1][:, 0:16]
            P.op("dve", lambda e: e.tensor_tensor(out=dm, in0=M2[:, :], in1=M1[:, :], op=ALU.subtract),
                 reads=[RT], writes=[RSM[0]])
            P.op("act", lambda e: e.activation(out=dm, in_=dm, func=AF.Sigmoid), writes=[RSM[0]])
            P.op("dve", lambda e: e.tensor_scalar(out=g1, in0=dm, scalar1=-1.0, scalar2=1.0,
                                                  op0=ALU.mult, op1=ALU.add),
                 reads=[RSM[0]], writes=[RSM[1]])
            for i in range(NT):
                P.op("dve", lambda e: e.tensor_scalar(out=gate[:, i, :], in0=eq1[:, i, :],
                                                      scalar1=g1[:, i:i + 1], scalar2=None, op0=ALU.mult),
                     reads=[RT, RSM[1]], writes=[GATE])
                P.op("dve", lambda e: e.scalar_tensor_tensor(out=gate[:, i, :], in0=eq2[:, i, :],
                                                             scalar=dm[:, i:i + 1], in1=gate[:, i, :],
                                                             op0=ALU.mult, op1=ALU.add),
                     reads=[RT, RSM[0]], writes=[GATE])

        def load_ln(g_ap, b_ap):
            P.dma("sp", d_ln, Gt[:], g_ap.partition_broadcast(128), writes=[GB])
            P.dma("sp", d_ln, Bt[:], b_ap.partition_broadcast(128), writes=[GB])

        def load_router(w_ap):
            if DBG.get("nordma"):
                P.op("dve", lambda e: e.memset(RTR["wr32"][:], 0.01), writes=[RTR["WR"]])
            else:
                P.dma("sp", d_misc, RTR["wr32"][:], w_ap.rearrange("(c p) e -> p c e", p=128), writes=[RTR["WR"]])

        def ln_pipeline(tiles, hmm, hsrc, tps, with_router=False):
            tps = tps if isinstance(tps, (list, tuple)) else [tps]
            n = len(tiles)

            def p1(t):
                i = tiles[t]
                k = i % 2
                xt = x_tm[:, i, :]
                if hsrc is not None:
                    hmm(i)
                    P.op("dve", lambda e: e.scalar_tensor_tensor(out=xt, in0=xt, scalar=ALPHA, in1=PS[hsrc][:, :],
                                                                 op0=ALU.mult, op1=ALU.add),
                         reads=PB[hsrc], writes=[X[i]])
                P.op("dve", lambda e: e.bn_stats(out=lnst[k][:, 0, :], in_=x_tm[:, i, 0:512]),
                     reads=[X[i]], writes=[LNS[k]])
                P.op("dve", lambda e: e.bn_stats(out=lnst[k][:, 1, :], in_=x_tm[:, i, 512:1024]),
                     reads=[X[i]], writes=[LNS[k]])
                mv = lnmv[k]
                P.op("dve", lambda e: e.bn_aggr(out=mv[:, 0:2], in_=lnst[k][:].rearrange("p a b -> p (a b)")),
                     reads=[LNS[k]], writes=[LNM[k]])
                P.op("dve", lambda e: e.tensor_scalar(out=mv[:, 2:3], in0=mv[:, 1:2], scalar1=LN_EPS, scalar2=None,
                                                      op0=ALU.add), reads=[LNM[k]], writes=[LNM[k]])
                P.op("act", lambda e: e.activation(out=mv[:, 3:4], in_=mv[:, 2:3], func=AF.Sqrt),
                     reads=[LNM[k]], writes=[LNM[k]])

            def p2(t):
                i = tiles[t]
                k = i % 2
                xt = x_tm[:, i, :]
                mv = lnmv[k]
                P.op("dve", lambda e: e.reciprocal(out=mv[:, 4:5], in_=mv[:, 3:4]), reads=[LNM[k]], writes=[LNM[k]])
                P.op("dve", lambda e: e.tensor_scalar(out=mv[:, 5:6], in0=mv[:, 0:1], scalar1=mv[:, 4:5], scalar2=-1.0,
                                                      op0=ALU.mult, op1=ALU.mult), reads=[LNM[k]], writes=[LNM[k]])
                P.op("act", lambda e: e.activation(out=xt, in_=xt, func=AF.Identity, scale=mv[:, 4:5], bias=mv[:, 5:6]),
                     reads=[LNM[k]], writes=[X[i]])

            def p3(t):
                i = tiles[t]
                xt = x_tm[:, i, :]
                P.op("dve", lambda e: e.tensor_tensor(out=xt, in0=xt, in1=Gt[:], op=ALU.mult),
                     reads=[GB], writes=[X[i]])
                P.op("pool", lambda e: e.tensor_tensor(out=xt, in0=xt, in1=Bt[:], op=ALU.add),
                     reads=[GB], writes=[X[i]])

            def p4(t):
                i = tiles[t]
                emit_xT(i, tps[t % len(tps)], with_router)

            for t in range(n + 3):
                if t < n:
                    p1(t)
                if 0 <= t - 1 < n:
                    p2(t - 1)
                if 0 <= t - 2 < n:
                    p3(t - 2)
                if 0 <= t - 3 < n:
                    p4(t - 3)

        for i in range(NT):
            emit_xT(i, 2 + (i % 2))

        def ffn_stage(wgu_aps, wd_aps, use_gate, g_ap, b_ap):
            with ExitStack() as st:
                def sbl(name, shape, dt):
                    return st.enter_context(nc.sbuf_tensor(uname(name), shape, dt))
                WG = [sbl(f"f_wg{i}", [128, NCH, 512], BF16) for i in range(2)]
                WU = [sbl(f"f_wu{i}", [128, NCH, 512], BF16) for i in range(2)]
                WD = [sbl(f"f_wd{i}", [128, 4, D], BF16) for i in range(2)]
                WGB = [[Buf() for _ in range(4)] for _ in range(2)]
                WDB = [Buf(), Buf()]
                hT = [sbl(f"f_hT{i}", [128, 4, 512], BF16) for i in range(2)]
                HB = [Buf(), Buf()]
                sg = [sbl(f"f_sg{i}", [128, 512], F32) for i in range(2)]
                SG = [Buf(), Buf()]

                load_ln(g_ap, b_ap)
                for i in range(NT):
                    P.op("act", lambda e: e.activation(out=x_tm[:, i, :], in_=x_tm[:, i, :], func=AF.Copy, scale=ALPHA),
                         writes=[X[i]])
                groups = [(e_, G) for e_ in range(len(wgu_aps)) for G in range(7)]

                def load_w(gi):
                    e_, G = groups[gi]
                    k = gi % 2
                    wgu = wgu_aps[e_].rearrange("(c p) n -> p c n", p=128)
                    wdn = wd_aps[e_].rearrange("(j p) n -> p j n", p=128)
                    if gi == 0:
                        for jj in range(4):
                            c0 = G * 512 + jj * 128
                            P.dma("pool", d_w[k], WG[k][:, :, jj * 128:(jj + 1) * 128], wgu[:, :, c0:c0 + 128],
                                  writes=[WGB[k][jj]])
                            P.dma("pool", d_w[k], WU[k][:, :, jj * 128:(jj + 1) * 128],
                                  wgu[:, :, DFF + c0:DFF + c0 + 128], writes=[WGB[k][jj]])
                    else:
                        P.dma("pool", d_w[k], WG[k][:], wgu[:, :, G * 512:(G + 1) * 512], writes=WGB[k])
                        P.dma("pool", d_w[k], WU[k][:], wgu[:, :, DFF + G * 512:DFF + (G + 1) * 512], writes=WGB[k])
                    P.dma("pool", d_w[k], WD[k][:], wdn[:, G * 4:(G + 1) * 4, :], writes=[WDB[k]])

                steps = [(gi, tb) for gi in range(len(groups)) for tb in range(4)]
                cnt = [0]

                def gu(s):
                    gi, tb = steps[s]
                    k = gi % 2
                    hk = s % 2
                    xin = [XT[tb * 4 + t] for t in range(4)]
                    for jj in range(4):
                        q = cnt[0] % 2
                        cnt[0] += 1
                        P.mm(PS[q][:, 0:512],
                             [(WG[k][:, c, jj * 128:(jj + 1) * 128], xT[:, c, tb * 512:(tb + 1) * 512])
                              for c in range(NCH)], reads=[WGB[k][jj]] + xin, writes=[PB[q][0]])
                        P.mm(PS[q][:, 512:1024],
                             [(WU[k][:, c, jj * 128:(jj + 1) * 128], xT[:, c, tb * 512:(tb + 1) * 512])
                              for c in range(NCH)], reads=[WGB[k][jj]] + xin, writes=[PB[q][1]])
                        P.op("act", lambda e: e.activation(out=sg[q][:], in_=PS[q][:, 0:512], func=AF.Silu),
                             reads=[PB[q][0]], writes=[SG[q]])
                        P.op("dve", lambda e: e.tensor_tensor(out=hT[hk][:, jj, :], in0=sg[q][:],
                                                              in1=PS[q][:, 512:1024], op=ALU.mult),
                             reads=[SG[q], PB[q][1]], writes=[HB[hk]])

                def down(s):
                    gi, tb = steps[s]
                    e_, G = groups[gi]
                    k = gi % 2
                    hk = s % 2
                    for tt in range(4):
                        i = tb * 4 + tt
                        y = 2 + (tt % 2)
                        for half in range(2):
                            P.mm(PS[y][:, half * 512:(half + 1) * 512],
                                 [(hT[hk][:, jj, tt * 128:(tt + 1) * 128], WD[k][:, jj, half * 512:(half + 1) * 512])
                                  for jj in range(4)], reads=[WDB[k], HB[hk]], writes=[PB[y][half]])
                        sc = gate[:, i, e_:e_ + 1] if use_gate else 1.0
                        rd = PB[y] + ([GATE] if use_gate else [])
                        P.op("dve", lambda e: e.scalar_tensor_tensor(out=x_tm[:, i, :], in0=PS[y][:, :], scalar=sc,
                                                                     in1=x_tm[:, i, :], op0=ALU.mult, op1=ALU.add),
                             reads=rd, writes=[X[i]])

                def tail_ln(s_done):
                    gi_d, tb_d = steps[s_done]
                    if gi_d == len(groups) - 1:
                        ln_pipeline([tb_d * 4 + t for t in range(4)], None, None, [2, 3], False)

                load_w(0)
                for s in range(len(steps)):
                    gi, tb = steps[s]
                    gu(s)
                    if s > 0:
                        down(s - 1)
                        tail_ln(s - 1)
                    if tb == 0 and gi + 1 < len(groups):
                        load_w(gi + 1)
                down(len(steps) - 1)
                tail_ln(len(steps) - 1)
                return

        def ffn_ln():
            ln_pipeline(list(range(NT)), None, None, [2, 3], False)

        def lru_stage(j, li, router_w):
            with ExitStack() as st:
                def sbl(name, shape, dt):
                    return st.enter_context(nc.sbuf_tensor(uname(name), shape, dt))
                praw = sbl("l_praw", [64, 128], F32)
                prm = sbl("l_prm", [128, 64], F32)
                prh = sbl("l_prh", [128, 16], F32)
                nsp = sbl("l_nsp", [128, 8], F32)
                nsph = sbl("l_nsph", [128, 8], F32)
                PRM = Buf()
                wout = sbl("l_wout", [128, NCH, D], BF16)
                WOUT = Buf()
                wgt = sbl("l_wg", [128, 8, 256], F32)
                WGT = Buf()
                win = sbl("l_win", [128, NCH, 2 * D], BF16)
                WINQ = [Buf() for _ in range(4)]
                mT = [sbl("l_mT0", [128, NCH, 512], BF16)]
                MT = [Buf()]
                halo = sbl("l_halo", [128, 8, 4], F32)
                hlast = sbl("l_hlast", [128, 8], F32)
                HALO = [Buf() for _ in range(8)]
                HL = [Buf() for _ in range(8)]

                def tmp2(name, shape, dt):
                    return [sbl(f"{name}{i}", shape, dt) for i in range(2)], [Buf(), Buf()]
                yg = [sbl(f"l_yg{i}", [128, 512], BF16) for i in range(3)]
                YG = [Buf(), Buf(), Buf()]
                rec, REC = tmp2("l_rec", [128, 516], F32)
                xr, XR = tmp2("l_xr", [128, 512], F32)
                ra, RA = tmp2("l_ra", [128, 512], F32)
                it, IT = tmp2("l_it", [128, 512], F32)
                mu, MU = tmp2("l_mu", [128, 512], F32)
                hh, HH = tmp2("l_hh", [128, 512], F32)

                load_ln(dram["ln_mix_g"][li], dram["ln_mix_b"][li])
                if router_w is not None:
                    alloc_router(sbl)
                    load_router(router_w)
                PRAW = Buf()
                P.dma("sp", d_misc, praw[0:32, :], dram["lru_conv_w"][j].rearrange("k (c p) -> (k c) p", p=128),
                      writes=[PRAW])
                P.dma("sp", d_misc, praw[32:40, :], dram["lru_conv_b"][j].rearrange("(c p) -> c p", p=128),
                      writes=[PRAW])
                P.dma("sp", d_misc, praw[40:48, :], dram["lru_lambda"][j].rearrange("(c p) -> c p", p=128),
                      writes=[PRAW])
                P.dma("sp", d_misc, praw[48:64, :], dram["lru_b_gates"][j].rearrange("n (h p) -> (n h) p", p=128),
                      writes=[PRAW])
                winv = dram["lru_w_in"][j].rearrange("(c p) n -> p c n", p=128)
                for q4 in (0, 2, 1, 3):
                    P.dma("pool", d_big, win[:, :, q4 * 512:(q4 + 1) * 512], winv[:, :, q4 * 512:(q4 + 1) * 512],
                          writes=[WINQ[q4]])
                P.dma("sp", d_big, wgt[:], dram["lru_w_gates"][j].rearrange("n p g -> p n g"), writes=[WGT])
                P.dma("pool", d_big, wout[:], dram["lru_w_out"][j].rearrange("(c p) n -> p c n", p=128),
                      writes=[WOUT])
                P.deps("pe", [PRAW, IDB], PB[3])
                ins = nc.tensor.transpose(out=PS[3][:, 0:64], in_=praw[:, :], identity=ident[0:64, 0:64])
                P.cnt["pe"] += 1
                ins.then_inc(P.sem["pe"], 1)
                tk = Tk(P.sem["pe"], P.cnt["pe"], "pe")
                P.mark(tk, [PRAW, IDB], PB[3])
                P.op("dve", lambda e: e.tensor_copy(out=prm[:], in_=PS[3][:, 0:64]), reads=PB[3], writes=[PRM])
                P.op("dve", lambda e: e.tensor_scalar(out=prh[:], in0=prm[:, 48:64], scalar1=0.5, scalar2=None,
                                                      op0=ALU.mult), writes=[PRM])
                P.op("act", lambda e: e.activation(out=nsp[:], in_=prm[:, 40:48], func=AF.Exp, scale=-1.0),
                     reads=[PRM], writes=[PRM])
                P.op("act", lambda e: e.activation(out=nsp[:], in_=nsp[:], func=AF.Ln, bias=1.0),
                     reads=[PRM], writes=[PRM])
                P.op("dve", lambda e: e.tensor_scalar(out=nsp[:], in0=nsp[:], scalar1=-8.0, scalar2=None,
                                                      op0=ALU.mult), reads=[PRM], writes=[PRM])
                P.op("dve", lambda e: e.tensor_scalar(out=nsph[:], in0=nsp[:], scalar1=0.5, scalar2=None,
                                                      op0=ALU.mult), writes=[PRM])
                P.op("dve", lambda e: e.memset(halo[:], 0.0), writes=HALO)
                P.op("dve", lambda e: e.memset(hlast[:], 0.0), writes=HL)

                steps = [(tb, c) for tb in range(4) for c in range(8)]

                def mm_gr(s):
                    tb, c = steps[s]
                    q = s % 2
                    xin = [XT[tb * 4 + t] for t in range(4)]
                    t0 = tb * 512
                    P.mm(PS[q][:, 0:512], [(win[:, cc, c * 128:(c + 1) * 128], xT[:, cc, t0:t0 + 512])
                                           for cc in range(NCH)], reads=[WINQ[c // 4]] + xin, writes=[PB[q][0]])
                    P.mm(PS[q][:, 512:1024], [(win[:, cc, D + c * 128:D + (c + 1) * 128], xT[:, cc, t0:t0 + 512])
                                              for cc in range(NCH)], reads=[WINQ[2 + c // 4]] + xin, writes=[PB[q][1]])

                def part_a(s):
                    tb, c = steps[s]
                    k = s % 2
                    q = s % 2
                    P.op("act", lambda e: e.activation(out=yg[s % 3][:], in_=PS[q][:, 0:512], func=AF.Gelu_apprx_tanh),
                         reads=[PB[q][0]], writes=[YG[s % 3]])
                    P.op("pool", lambda e: e.tensor_copy(out=rec[k][:, 0:4], in_=halo[:, c, :]),
                         reads=[HALO[c]], writes=[REC[k]])
                    P.op("dve", lambda e: e.tensor_copy(out=rec[k][:, 4:516], in_=PS[q][:, 512:1024]),
                         reads=[PB[q][1]], writes=[REC[k]])

                def c_dve(s):
                    tb, c = steps[s]
                    k = s % 2
                    q = s % 2
                    P.op("pool", lambda e: e.tensor_copy(out=halo[:, c, :], in_=rec[k][:, 512:516]),
                         reads=[REC[k]], writes=[HALO[c]])
                    P.op("dve", lambda e: e.tensor_scalar(out=xr[k][:], in0=rec[k][:, 4:516],
                                                          scalar1=prm[:, 24 + c:25 + c], scalar2=prm[:, 32 + c:33 + c],
                                                          op0=ALU.mult, op1=ALU.add),
                         reads=[REC[k], PRM], writes=[XR[k]])
                    for kk in range(3):
                        sh = 3 - kk
                        P.op("dve", lambda e: e.scalar_tensor_tensor(out=xr[k][:], in0=rec[k][:, 4 - sh:516 - sh],
                                                                     scalar=prm[:, kk * 8 + c:kk * 8 + c + 1],
                                                                     in1=xr[k][:], op0=ALU.mult, op1=ALU.add),
                             reads=[REC[k], PRM], writes=[XR[k]])

                def c_act(s):
                    tb, c = steps[s]
                    k = s % 2
                    q = s % 2
                    P.mm(PS[q][:, 0:512], [(wgt[:, c, 0:128], xr[k][:])], reads=[WGT, XR[k]], writes=[PB[q][0]])
                    P.mm(PS[q][:, 512:1024], [(wgt[:, c, 128:256], xr[k][:])], reads=[WGT, XR[k]],
                         writes=[PB[q][1]])

                def e_act(s):
                    tb, c = steps[s]
                    k = s % 2
                    q = s % 2
                    P.op("act", lambda e: e.activation(out=ra[k][:], in_=PS[q][:, 0:512], func=AF.Tanh, scale=0.5,
                                                       bias=prh[:, 2 * c:2 * c + 1]),
                         reads=[PB[q][0], PRM], writes=[RA[k]])
                    P.op("act", lambda e: e.activation(out=it[k][:], in_=PS[q][:, 512:1024], func=AF.Tanh, scale=0.5,
                                                       bias=prh[:, 2 * c + 1:2 * c + 2]),
                         reads=[PB[q][1], PRM], writes=[IT[k]])
                    P.op("act", lambda e: e.activation(out=mu[k][:], in_=ra[k][:], func=AF.Exp, scale=nsp[:, c:c + 1],
                                                       bias=nsp[:, c:c + 1]),
                         reads=[RA[k], PRM], writes=[MU[k]])
                    P.op("act", lambda e: e.activation(out=ra[k][:], in_=ra[k][:], func=AF.Exp, scale=nsph[:, c:c + 1],
                                                       bias=nsph[:, c:c + 1]),
                         reads=[PRM], writes=[RA[k]])
                    P.op("act", lambda e: e.activation(out=mu[k][:], in_=mu[k][:], func=AF.Ln, scale=-1.0, bias=1.0),
                         writes=[MU[k]])
                    P.op("act", lambda e: e.activation(out=mu[k][:], in_=mu[k][:], func=AF.Exp, scale=0.5),
                         writes=[MU[k]])

                def f_dve(s):
                    tb, c = steps[s]
                    k = s % 2
                    q = s % 2
                    P.op("dve", lambda e: e.scalar_tensor_tensor(out=it[k][:], in0=it[k][:], scalar=1.0, in1=xr[k][:],
                                                                 op0=ALU.add, op1=ALU.mult),
                         reads=[XR[k]], writes=[IT[k]])
                    P.op("dve", lambda e: e.scalar_tensor_tensor(out=mu[k][:], in0=mu[k][:], scalar=0.5, in1=it[k][:],
                                                                 op0=ALU.mult, op1=ALU.mult),
                         reads=[IT[k]], writes=[MU[k]])
                    P.op("dve", lambda e: e.tensor_tensor_scan(out=hh[k][:], data0=ra[k][:], data1=mu[k][:],
                                                               initial=hlast[:, c:c + 1], op0=ALU.mult, op1=ALU.add),
                         reads=[RA[k], MU[k], HL[c]], writes=[HH[k]])
                    P.op("dve", lambda e: e.tensor_copy(out=hlast[:, c:c + 1], in_=hh[k][:, 511:512]),
                         reads=[HH[k]], writes=[HL[c]])
                    P.op("pool", lambda e: e.tensor_tensor(out=mT[0][:, c, :], in0=yg[s % 3][:], in1=hh[k][:], op=ALU.mult),
                         reads=[YG[s % 3], HH[k]], writes=[MT[0]])

                mm_gr(0)
                part_a(0)
                c_dve(0)
                c_act(0)
                for s, (tb, c) in enumerate(steps):
                    if s + 1 < len(steps):
                        mm_gr(s + 1)
                        part_a(s + 1)
                        c_dve(s + 1)
                    e_act(s)
                    if s + 1 < len(steps):
                        c_act(s + 1)
                    f_dve(s)
                    if c == 7:
                        tiles = [tb * 4 + tt for tt in range(4)]

                        def hmm(i):
                            tt = i % 4
                            for half in range(2):
                                P.mm(PS[2][:, half * 512:(half + 1) * 512],
                                     [(mT[0][:, cc, tt * 128:(tt + 1) * 128], wout[:, cc, half * 512:(half + 1) * 512])
                                      for cc in range(NCH)], reads=[MT[0], WOUT], writes=[PB[2][half]])
                        ln_pipeline(tiles, hmm, 2, 3, router_w is not None)
                if router_w is not None:
                    finish_router()

        def pool_stage(li, router_w):
            with ExitStack() as st:
                def sbl(name, shape, dt):
                    return st.enter_context(nc.sbuf_tensor(uname(name), shape, dt))
                pT = sbl("p_pT", [128, NCH, S], BF16)
                PT_ = [Buf() for _ in range(NCH)]
                pw32 = sbl("p_w32", [128, 4, 2, 256], F32)
                pwb = sbl("p_wb", [128, 4, 2, 256], BF16)
                scb = sbl("p_scb", [128, D], F32)
                PW = Buf()
                rc16 = sbl("p_rc", [128, 16], F32)
                RC = Buf()
                A = [sbl(f"p_A{i}", [128, S], F32) for i in range(2)]
                Bq = [sbl(f"p_B{i}", [128, S], F32) for i in range(2)]
                AB = [Buf(), Buf()]
                tsm = [sbl(f"p_t{i}", [128, 16], F32) for i in range(2)]

                load_ln(dram["ln_mix_g"][li], dram["ln_mix_b"][li])
                if router_w is not None:
                    alloc_router(sbl)
                    load_router(router_w)
                P.dma("sp", d_misc, pw32[:], dram["pool_w"][0].rearrange("g (k p) d -> p g k d", p=128), writes=[PW])
                P.dma("sp", d_misc, scb[:], dram["pool_scale"][0].partition_broadcast(128), writes=[PW])
                for g in range(4):
                    for kc in range(2):
                        P.op("dve", lambda e: e.tensor_tensor(out=pwb[:, g, kc, :], in0=pw32[:, g, kc, :],
                                                              in1=scb[:, g * 256:(g + 1) * 256], op=ALU.mult),
                             reads=[PW], writes=[PW])
                for t in range(16):
                    P.op("dve", lambda e: e.memset(rc16[:, t:t + 1], 1.0 / (t + 1)), writes=[RC])
                for c in range(NCH):
                    g = c // 2
                    wl = 2 ** (g + 1)
                    k = c % 2
                    eng = "dve"
                    xc = xT[:, c, :]
                    srcb = xc
                    bufs2 = [A[k], Bq[k]]
                    bi = 0
                    sh = 1
                    while sh < wl:
                        dst = bufs2[bi]
                        P.op(eng, lambda e: e.tensor_tensor(out=dst[:, sh:S], in0=srcb[:, sh:S], in1=srcb[:, 0:S - sh],
                                                            op=ALU.add), reads=XT, writes=[AB[k]])
                        P.op(eng, lambda e: e.tensor_copy(out=dst[:, 0:sh], in_=srcb[:, 0:sh]), reads=XT, writes=[AB[k]])
                        srcb = dst
                        bi ^= 1
                        sh *= 2
                    cur = srcb
                    P.op("dve", lambda e: e.scalar_tensor_tensor(out=pT[:, c, :], in0=cur[:, :], scalar=1.0 / wl,
                                                                 in1=xc, op0=ALU.mult, op1=ALU.subtract),
                         reads=[AB[k]] + XT, writes=[PT_[c]])
                    if wl > 2:
                        n = wl - 1
                        P.op("dve", lambda e: e.tensor_tensor(out=tsm[k][:, 0:n], in0=cur[:, 0:n], in1=rc16[:, 0:n],
                                                              op=ALU.mult), reads=[AB[k], RC], writes=[AB[k]])
                        P.op("dve", lambda e: e.tensor_tensor(out=pT[:, c, 0:n], in0=tsm[k][:, 0:n], in1=xT[:, c, 0:n],
                                                              op=ALU.subtract), reads=[AB[k]], writes=[PT_[c]])
                    else:
                        P.op("dve", lambda e: e.memset(pT[:, c, 0:1], 0.0), writes=[PT_[c]])
                def hmm(i):
                    for g in range(4):
                        half = g // 2
                        P.mm(PS[2][:, g * 256:(g + 1) * 256],
                             [(pT[:, 2 * g + kc, i * 128:(i + 1) * 128], pwb[:, g, kc, :]) for kc in range(2)],
                             reads=[PT_[2 * g], PT_[2 * g + 1], PW], writes=[PB[2][half]])
                ln_pipeline(list(range(NT)), hmm, 2, 3, router_w is not None)
                if router_w is not None:
                    finish_router()

        def fox_stage(li):
            wqkvf = dram["fox_w_qkvf"][0].rearrange("(c p) n -> p c n", p=128)
            with ExitStack() as st0:
                oT = st0.enter_context(nc.sbuf_tensor(uname("a_oT"), [128, NCH, S], BF16))
                OT = Buf()
                load_ln(dram["ln_mix_g"][li], dram["ln_mix_b"][li])
                with ExitStack() as st:
                    def sbl(name, shape, dt):
                        return st.enter_context(nc.sbuf_tensor(uname(name), shape, dt))
                    wf = sbl("a_wf", [128, NCH, 16], BF16)
                    WF = Buf()
                    nbf = sbl("a_nbf", [16, 1], F32)
                    NBF = Buf()
                    ones16 = sbl("a_ones", [16, 512], F32)
                    spT = sbl("a_spT", [16, 512], F32)
                    SPT = Buf()
                    Fneg = sbl("a_Fneg", [16, S], F32)
                    FN = Buf()
                    Fk = sbl("a_Fk", [128, NT, 16], F32)
                    FK = Buf()
                    neg1 = sbl("a_neg1", [16, 128], F32)
                    sel = [sbl(f"a_sel{i}", [16, 128], F32) for i in range(2)]
                    SEL = [Buf(), Buf()]
                    NEG1 = Buf()
                    wq2 = [sbl(f"a_wq{i}", [128, NCH, 128], BF16) for i in range(2)]
                    wk2 = [sbl(f"a_wk{i}", [128, NCH, 128], BF16) for i in range(2)]
                    wv2 = [sbl(f"a_wv{i}", [128, NCH, 128], BF16) for i in range(2)]
                    WQ2 = [Buf(), Buf()]

                    def load_qkv(cq):
                        kq = cq % 2
                        P.dma("pool", d_w[0], wq2[kq][:], wqkvf[:, :, cq * 128:(cq + 1) * 128], writes=[WQ2[kq]])
                        P.dma("pool", d_w[0], wk2[kq][:], wqkvf[:, :, D + cq * 128:D + (cq + 1) * 128],
                              writes=[WQ2[kq]])
                        P.dma("pool", d_w[0], wv2[kq][:], wqkvf[:, :, 2 * D + cq * 128:2 * D + (cq + 1) * 128],
                              writes=[WQ2[kq]])
                    qT = sbl("a_qT", [128, S], BF16)
                    kT = sbl("a_kT", [128, S], BF16)
                    QK = Buf()
                    vc = sbl("a_vc", [128, NT, 2, 65], BF16)
                    VC = Buf()
                    Fq = [sbl(f"a_Fq{i}", [128, 512], F32) for i in range(2)]
                    FQ = [Buf(), Buf()]
                    tmp = [sbl(f"a_tmp{i}", [128, 512], F32) for i in range(4)]
                    TMP = [Buf() for _ in range(4)]
                    pt = [sbl(f"a_pt{i}", [128, 512], BF16) for i in range(4)]
                    PTB = [Buf() for _ in range(4)]
                    SPS = [(PS[0], 0, PB[0][0]), (PS[0], 512, PB[0][1]), (PS[3], 512, PB[3][1]), (PS[3], 0, PB[3][0])]
                    NS = len(SPS)
                    otm = sbl("a_otm", [128, NT, 128], F32)
                    OTM = Buf()
                    rs = sbl("a_rs", [128, 8], F32)
                    RS = Buf()

                    P.dma("pool", d_big, wf[:], wqkvf[:, :, 3 * D:3 * D + 16], writes=[WF])
                    P.dma("sp", d_misc, nbf[:], dram["fox_b_f"][0].rearrange("(h o) -> h o", o=1), writes=[NBF])
                    P.op("dve", lambda e: e.tensor_scalar(out=nbf[:], in0=nbf[:], scalar1=-1.0, scalar2=None,
                                                          op0=ALU.mult), writes=[NBF])
                    P.op("pool", lambda e: e.memset(ones16[:], 1.0), writes=[SPT])
                    P.op("pool", lambda e: e.memset(neg1[:], -1.0), writes=[NEG1])
                    P.op("pool", lambda e: e.memset(vc[:], 1.0), writes=[VC])
                    for tb in range(4):
                        xin = [XT[tb * 4 + t] for t in range(4)]
                        P.mm(PS[3][0:16, 0:512], [(wf[:, c, :], xT[:, c, tb * 512:(tb + 1) * 512]) for c in range(NCH)],
                             reads=[WF] + xin, writes=[PB[3][0]])
                        P.op("act", lambda e: e.activation(out=spT[:], in_=PS[3][0:16, 0:512],
                                                           func=AF.Exp, scale=-1.0, bias=nbf[:, 0:1]),
                             reads=[PB[3][0], NBF], writes=[SPT])
                        P.op("act", lambda e: e.activation(out=spT[:], in_=spT[:], func=AF.Ln, bias=1.0), writes=[SPT])
                        init = 0.0 if tb == 0 else Fneg[:, tb * 512 - 1:tb * 512]
                        P.op("dve", lambda e: e.tensor_tensor_scan(out=Fneg[:, tb * 512:(tb + 1) * 512], data0=ones16[:],
                                                                   data1=spT[:], initial=init,
                                                                   op0=ALU.mult, op1=ALU.add),
                             reads=[SPT], writes=[FN])
                    P.deps("pe", [FN, IDB], PB[3])
                    ins = None
                    for i in range(NT):
                        ins = nc.tensor.transpose(out=PS[3][:, i * 16:(i + 1) * 16], in_=Fneg[:, i * 128:(i + 1) * 128],
                                                  identity=ident[0:16, 0:16])
                    P.cnt["pe"] += 1
                    ins.then_inc(P.sem["pe"], 1)
                    tk = Tk(P.sem["pe"], P.cnt["pe"], "pe")
                    P.mark(tk, [FN, IDB], PB[3])
                    P.op("dve", lambda e: e.tensor_copy(out=Fk[:].rearrange("p i h -> p (i h)"), in_=PS[3][:, 0:256]),
                         reads=PB[3], writes=[FK])

                    fqc = [0]
                    load_qkv(0)
                    for c in range(NCH):
                        wq, wk, wv, WQ = wq2[c % 2], wk2[c % 2], wv2[c % 2], WQ2[c % 2]
                        if c + 1 < NCH:
                            load_qkv(c + 1)
                        for tb in range(4):
                            xin = [XT[tb * 4 + t] for t in range(4)]
                            P.mm(PS[3][:, 0:512], [(wq[:, cc, :], xT[:, cc, tb * 512:(tb + 1) * 512]) for cc in range(NCH)],
                                 reads=[WQ] + xin, writes=[PB[3][0]])
                            P.op("act", lambda e: e.activation(out=qT[:, tb * 512:(tb + 1) * 512], in_=PS[3][:, 0:512],
                                                               func=AF.Copy, scale=0.125), reads=[PB[3][0]], writes=[QK])
                            P.mm(PS[3][:, 512:1024],
                                 [(wk[:, cc, :], xT[:, cc, tb * 512:(tb + 1) * 512]) for cc in range(NCH)],
                                 reads=[WQ] + xin, writes=[PB[3][1]])
                            P.op("dve", lambda e: e.tensor_copy(out=kT[:, tb * 512:(tb + 1) * 512], in_=PS[3][:, 512:1024]),
                                 reads=[PB[3][1]], writes=[QK])
                        for i4 in range(4):
                            bk = i4 % 2
                            rdl = [WQ] + XT[i4 * 4:(i4 + 1) * 4]
                            P.deps("pe", rdl, [PB[3][bk]])
                            ins = None
                            for t in range(4):
                                i = i4 * 4 + t
                                for cc in range(NCH):
                                    ins = nc.tensor.matmul(PS[3][:, bk * 512 + t * 128:bk * 512 + (t + 1) * 128],
                                                           lhsT=xT[:, cc, i * 128:(i + 1) * 128], rhs=wv[:, cc, :],
                                                           start=(cc == 0), stop=(cc == NCH - 1))
                            P.cnt["pe"] += 1
                            ins.then_inc(P.sem["pe"], 1)
                            tk = Tk(P.sem["pe"], P.cnt["pe"], "pe")
                            P.mark(tk, rdl, [PB[3][bk]])
                            P.op("act", lambda e: e.activation(
                                out=vc[:, i4 * 4:(i4 + 1) * 4, :, 0:64],
                                in_=PS[3][:, bk * 512:(bk + 1) * 512].rearrange("p (t h d) -> p t h d", t=4, h=2),
                                func=AF.Copy), reads=[PB[3][bk]], writes=[VC])
                        for hh_ in range(2):
                            h = 2 * c + hh_
                            hb = hh_ * 64
                            sk_ = h % 2
                            P.op("pool", lambda e: e.affine_select(out=sel[sk_][:], in_=neg1[:], pattern=[[0, 128]],
                                                                   compare_op=ALU.is_equal, fill=0.0, base=-h,
                                                                   channel_multiplier=1),
                                 reads=[NEG1], writes=[SEL[sk_]])
                            iters = [(qb, j) for qb in range(4) for j in range(4 * qb + 4)]
                            fqbuf = {}

                            def emit_fq(qb):
                                fk_ = fqc[0] % 2
                                fqc[0] += 1
                                fqbuf[qb] = fk_
                                P.mm(PS[3][:, 0:512], [(sel[sk_][:], Fneg[:, qb * 512:(qb + 1) * 512])],
                                     reads=[SEL[sk_], FN], writes=[PB[3][0]])
                                P.op("act", lambda e: e.activation(out=Fq[fk_][:], in_=PS[3][:, 0:512], func=AF.Copy),
                                     reads=[PB[3][0]], writes=[FQ[fk_]])

                            def geom(n):
                                qb, j = iters[n]
                                c0 = max(0, j - 4 * qb) * 128
                                return qb, j, c0, qb * 512 + c0, (qb + 1) * 512

                            def emit_S(n):
                                qb, j, c0, q0, q1 = geom(n)
                                sps, so, sbuf_ = SPS[n % NS]
                                P.mm(sps[:, so + c0:so + 512],
                                     [(kT[hb:hb + 64, j * 128:(j + 1) * 128], qT[hb:hb + 64, q0:q1])],
                                     reads=[QK], writes=[sbuf_])

                            def emit_soft(n):
                                qb, j, c0, q0, q1 = geom(n)
                                sk = n % NS
                                sps, so, sbuf_ = SPS[sk]
                                fk_ = fqbuf[qb]
                                P.op("dve", lambda e: e.tensor_tensor(out=tmp[sk][:, c0:512],
                                                                      in0=sps[:, so + c0:so + 512],
                                                                      in1=Fq[fk_][:, c0:512], op=ALU.add),
                                     reads=[sbuf_, FQ[fk_]], writes=[TMP[sk]])
                                P.op("act", lambda e: e.activation(out=pt[sk][:, c0:512], in_=tmp[sk][:, c0:512],
                                                                   func=AF.Exp, bias=Fk[:, j, h:h + 1]),
                                     reads=[TMP[sk], FK], writes=[PTB[sk]])
                                if j >= 4 * qb:
                                    P.op("pool", lambda e: e.affine_select(out=pt[sk][:, c0:c0 + 128],
                                                                           in_=pt[sk][:, c0:c0 + 128],
                                                                           pattern=[[1, 128]], compare_op=ALU.is_ge,
                                                                           fill=0.0, base=0, channel_multiplier=-1),
                                         writes=[PTB[sk]])

                            def emit_PV(n):
                                qb, j, c0, q0, q1 = geom(n)
                                sk = n % NS
                                for ii in range(max(0, j - 4 * qb), 4):
                                    i = 4 * qb + ii
                                    ob = 1 + ii // 2
                                    oh = ii % 2
                                    P.deps("pe", [PTB[sk], VC], [PB[ob][oh]] if j == 0 else [])
                                    ins = nc.tensor.matmul(PS[ob][:, oh * 512:oh * 512 + 65],
                                                           lhsT=pt[sk][:, ii * 128:(ii + 1) * 128],
                                                           rhs=vc[:, j, hh_, :], start=(j == 0), stop=(j == i))
                                    P.cnt["pe"] += 1
                                    ins.then_inc(P.sem["pe"], 1)
                                    tk = Tk(P.sem["pe"], P.cnt["pe"], "pe")
                                    P.mark(tk, [PTB[sk], VC], [PB[ob][oh]])

                            def emit_norm(qb):
                                for ii in range(4):
                                    i = 4 * qb + ii
                                    ob = 1 + ii // 2
                                    oh = ii % 2
                                    P.op("dve", lambda e: e.reciprocal(out=rs[:, ii:ii + 1],
                                                                       in_=PS[ob][:, oh * 512 + 64:oh * 512 + 65]),
                                         reads=[PB[ob][oh]], writes=[RS])
                                    P.op("dve", lambda e: e.tensor_scalar(out=otm[:, i, hb:hb + 64],
                                                                          in0=PS[ob][:, oh * 512:oh * 512 + 64],
                                                                          scalar1=rs[:, ii:ii + 1], scalar2=None,
                                                                          op0=ALU.mult),
                                         reads=[PB[ob][oh], RS], writes=[OTM])

                            emit_fq(0)
                            emit_S(0)
                            emit_S(1)
                            emit_S(2)
                            for n in range(len(iters)):
                                qb, j = iters[n]
                                if j == 0 and qb + 1 < 4:
                                    emit_fq(qb + 1)
                                emit_soft(n)
                                if n + 3 < len(iters):
                                    emit_S(n + 3)
                                emit_PV(n)
                                if j == 4 * qb + 3:
                                    emit_norm(qb)
                        for i4 in range(4):
                            bk = i4 % 2
                            P.deps("pe", [OTM, IDB], [PB[3][bk]])
                            ins = None
                            for t in range(4):
                                i = i4 * 4 + t
                                ins = nc.tensor.transpose(out=PS[3][:, bk * 512 + t * 128:bk * 512 + (t + 1) * 128],
                                                          in_=otm[:, i, :], identity=ident[:])
                            P.cnt["pe"] += 1
                            ins.then_inc(P.sem["pe"], 1)
                            tk = Tk(P.sem["pe"], P.cnt["pe"], "pe")
                            P.mark(tk, [OTM, IDB], [PB[3][bk]])
                            P.op("act", lambda e: e.activation(out=oT[:, c, i4 * 512:(i4 + 1) * 512],
                                                               in_=PS[3][:, bk * 512:(bk + 1) * 512], func=AF.Copy),
                                 reads=[PB[3][bk]], writes=[OT])
                    P.barrier()
                with nc.sbuf_tensor(uname("a_wo"), [128, NCH, D], BF16) as wo:
                    WO = Buf()
                    P.dma("pool", d_big, wo[:], dram["fox_w_o"][0].rearrange("(c p) n -> p c n", p=128), writes=[WO])
                    def hmm(i):
                        for half in range(2):
                            P.mm(PS[2][:, half * 512:(half + 1) * 512],
                                 [(oT[:, cc, i * 128:(i + 1) * 128], wo[:, cc, half * 512:(half + 1) * 512])
                                  for cc in range(NCH)], reads=[OT, WO], writes=[PB[2][half]])
                    ln_pipeline(list(range(NT)), hmm, 2, [3, 0], False)
                    P.barrier()

        for stg in plan:
            P.barrier()
            if stg.startswith("lru"):
                j = int(stg[3])
                li = 0 if j == 0 else 3
                lru_stage(j, li, dram["moe_router"][1] if li == 3 else None)
            elif stg == "poolnr":
                pool_stage(1, None)
            elif stg == "lnrouter":
                with ExitStack() as st:
                    def sbl(name, shape, dt):
                        return st.enter_context(nc.sbuf_tensor(uname(name), shape, dt))
                    load_ln(dram["ln_mix_g"][0], dram["ln_mix_b"][0])
                    alloc_router(sbl)
                    load_router(dram["moe_router"][0])
                    ln_pipeline(list(range(NT)), None, None, 3, True)
                    finish_router()
                    P.barrier()
            elif stg.startswith("pool"):
                pool_stage(1, dram["moe_router"][0])
            elif stg.startswith("fox"):
                fox_stage(2)
            elif stg.startswith("ffn"):
                j = int(stg[3])
                li = 0 if j == 0 else 2
                ffn_stage([dram["ffn_w_gu"][j]], [dram["ffn_w_down"][j]], False,
                          dram["ln_ffn_g"][li], dram["ln_ffn_b"][li])
            elif stg.startswith("moe"):
                j = int(stg[3])
                li = 1 if j == 0 else 3
                ffn_stage([dram["moe_w_gu"][j][e_] for e_ in range(NE)], [dram["moe_w_down"][j][e_] for e_ in range(NE)],
                          True, dram["ln_ffn_g"][li], dram["ln_ffn_b"][li])
            elif stg == "ldln":
                load_ln(dram["ln_mix_g"][0], dram["ln_mix_b"][0])
            elif stg == "lnonly":
                load_ln(dram["ln_mix_g"][0], dram["ln_mix_b"][0])
                ln_pipeline(list(range(NT)), None, None, [2, 3], False)
        P.barrier()
        ov = out_d.rearrange("(i p) d -> p i d", p=128)
        for i4 in range(4):
            P.dma("sp", d_out, ov[:, i4 * 4:(i4 + 1) * 4, :], x_tm[:, i4 * 4:(i4 + 1) * 4, :],
                  reads=X[i4 * 4:(i4 + 1) * 4])
        P.barrier()
    return nc


def run(inputs, plan=None, n_cores=8, trace=False):
    plan = FULL_PLAN if plan is None else plan
    nc = build_program(plan)
    x = np.ascontiguousarray(inputs["x"], dtype=np.float32)
    shared = {k: np.ascontiguousarray(inputs[k], dtype=np.float32) for k in INPUT_SHAPES
              if k in needed_inputs(plan)}
    in_maps = []
    for b in range(n_cores):
        m = dict(shared)
        m["x"] = x[b]
        in_maps.append(m)
    res = run_bass_kernel_spmd(nc, in_maps, core_ids=list(range(n_cores)), trace=trace)
    out = np.stack([r["out"] for r in res.results], axis=0)
    return out, res


def kernel(**inputs):
    out, _ = run(inputs)
    return out.astype(np.float32)
```
